# Optimizing a Trainium2 kernel written in Bass

```python
import functools
import jax, jax.numpy as jnp
from jax import lax
import numpy as np

D_MODEL = 1024
BATCH = 32
SEQ = 2048
DEPTH = 2
DEC_BATCH = 32
DEC_SEQ = 32
PAST_LEN = 1024

CHUNK = 64
Q_BLOCK = 128
D_PLE = 256
FOX_HEADS = 8
FOX_HEAD_DIM = 64
FOX_W = FOX_HEADS * FOX_HEAD_DIM
MLA_HEADS = 8
MLA_Q_RANK = 256
MLA_KV_RANK = 128
MLA_NOPE = 64
MLA_ROPE = 32
MLA_V = 64
MLA_W = MLA_HEADS * MLA_V
ROPE_THETA = 10000.0
D_FF = 2816
N_EXPERTS = 8
TOP_K = 2
D_FF_EXPERT = 2816
N_DENSE = (DEPTH + 1) // 2
N_MOE = DEPTH // 2
DN_ALPHA = (2 * DEPTH) ** 0.25
DN_BETA = (8 * DEPTH) ** -0.25
LN_EPS = 1e-5
RMS_EPS = 1e-6
NEG_INF = -1e30
IN_SIZES = (FOX_W, FOX_W, FOX_W, FOX_HEADS, MLA_Q_RANK, MLA_KV_RANK, MLA_ROPE, D_MODEL, D_MODEL)
IN_WIDTH = 3 * FOX_W + FOX_HEADS + MLA_Q_RANK + MLA_KV_RANK + MLA_ROPE + 2 * D_MODEL

kernel_name = 'hybrid_fox_mla_streaming_step'


def layer_norm(x, g, b):
    xf = x.astype(jnp.float32)
    mu = jnp.mean(xf, axis=-1, keepdims=True)
    var = jnp.mean(jnp.square(xf - mu), axis=-1, keepdims=True)
    y = (xf - mu) * lax.rsqrt(var + LN_EPS)
    return (y * g.astype(jnp.float32) + b.astype(jnp.float32)).astype(x.dtype)


def rms_norm(x, g):
    xf = x.astype(jnp.float32)
    y = xf * lax.rsqrt(jnp.mean(jnp.square(xf), axis=-1, keepdims=True) + RMS_EPS)
    return (y * g.astype(jnp.float32)).astype(x.dtype)


def rope(x, pos):
    r = x.shape[-1]
    half = r // 2
    inv = ROPE_THETA ** (-jnp.arange(half, dtype=jnp.float32) * 2.0 / r)
    ang = pos.astype(jnp.float32)[:, None] * inv[None, :]
    cos = jnp.cos(ang)[:, None, :]
    sin = jnp.sin(ang)[:, None, :]
    x1 = x[..., :half].astype(jnp.float32)
    x2 = x[..., half:].astype(jnp.float32)
    return jnp.concatenate([x1 * cos - x2 * sin, x2 * cos + x1 * sin], axis=-1).astype(x.dtype)


def split_cols(z):
    out, start = [], 0
    for n in IN_SIZES:
        out.append(z[..., start:start + n])
        start += n
    return out


def attend(q, k, v, q_pos, k_pos, q_cum, k_cum, chunk_causal):
    b, s, h, dk = q.shape
    qb = Q_BLOCK if s % Q_BLOCK == 0 else s
    nb = s // qb
    scale = dk ** -0.5
    k_grp = k_pos // CHUNK if chunk_causal else k_pos
    k_cum_t = None if k_cum is None else jnp.swapaxes(k_cum.astype(jnp.float32), 1, 2)

    def to_blocks(a):
        return jnp.swapaxes(a.reshape((b, nb, qb) + a.shape[2:]), 0, 1)

    def one_block(args):
        q_blk, pos_blk, cum_blk = args
        sc = jnp.einsum('bqhd,bkhd->bhqk', q_blk, k).astype(jnp.float32) * scale
        if cum_blk is not None:
            sc = sc + jnp.swapaxes(cum_blk.astype(jnp.float32), 1, 2)[..., None] - k_cum_t[:, :, None, :]
        q_grp = pos_blk // CHUNK if chunk_causal else pos_blk
        visible = k_grp[None, :] <= q_grp[:, None]
        sc = jnp.where(visible, sc, NEG_INF)
        pr = jax.nn.softmax(sc, axis=-1).astype(v.dtype)
        return jnp.einsum('bhqk,bkhd->bqhd', pr, v)

    xs = (to_blocks(q), q_pos.reshape(nb, qb), None if q_cum is None else to_blocks(q_cum))
    out = lax.map(one_block, xs)
    return jnp.swapaxes(out, 0, 1).reshape(b, s, h, v.shape[-1])


def token_mixers(x, past, w_in, b_fox_f, g_mla_cq, w_mla_qb, g_mla_ckv, w_mla_kvb, w_o_fox, w_o_mla, w_out):
    b, s, _ = x.shape
    n_past = 0 if past is None else past[0].shape[1]
    q_pos = n_past + jnp.arange(s)
    k_pos = jnp.arange(n_past + s)
    t = n_past + s
    fq, fk, fv, ff, cq, ckv, kr, ga, gb = split_cols(x @ w_in)
    fq = fq.reshape(b, s, FOX_HEADS, FOX_HEAD_DIM)
    fk = fk.reshape(b, s, FOX_HEADS, FOX_HEAD_DIM)
    fv = fv.reshape(b, s, FOX_HEADS, FOX_HEAD_DIM)
    logf = jax.nn.log_sigmoid((ff + b_fox_f).astype(jnp.float32))
    ckv = rms_norm(ckv, g_mla_ckv)
    kr = rope(kr[:, :, None, :], q_pos)[:, :, 0, :]
    qm = (rms_norm(cq, g_mla_cq) @ w_mla_qb).reshape(b, s, MLA_HEADS, MLA_NOPE + MLA_ROPE)
    qm = jnp.concatenate([qm[..., :MLA_NOPE], rope(qm[..., MLA_NOPE:], q_pos)], axis=-1)
    new_rows = (fk, fv, logf, ckv, kr)
    if past is None:
        fk_all, fv_all, logf_all, ckv_all, kr_all = new_rows
    else:
        fk_all, fv_all, logf_all, ckv_all, kr_all = [jnp.concatenate([c, n], axis=1) for c, n in zip(past, new_rows)]
    cum = jnp.cumsum(logf_all.astype(jnp.float32), axis=1)
    o_fox = attend(fq, fk_all, fv_all, q_pos, k_pos, cum[:, n_past:], cum, False)
    kv = (ckv_all @ w_mla_kvb).reshape(b, t, MLA_HEADS, MLA_NOPE + MLA_V)
    k_rope_h = jnp.broadcast_to(kr_all[:, :, None, :], (b, t, MLA_HEADS, MLA_ROPE)).astype(kv.dtype)
    km = jnp.concatenate([kv[..., :MLA_NOPE], k_rope_h], axis=-1)
    o_mla = attend(qm, km, kv[..., MLA_NOPE:], q_pos, k_pos, None, None, True)
    merged = (jax.nn.sigmoid(ga) * (o_fox.reshape(b, s, FOX_W) @ w_o_fox)
              + jax.nn.sigmoid(gb) * (o_mla.reshape(b, s, MLA_W) @ w_o_mla))
    return merged @ w_out, new_rows


def swiglu(x, w_gate, w_up, w_down):
    return (jax.nn.silu(x @ w_gate) * (x @ w_up)) @ w_down


def moe_swiglu(x, w_router, b_router, w_gate, w_up, w_down):
    logits = (x @ w_router + b_router).astype(jnp.float32)
    top_val, top_idx = lax.top_k(logits, TOP_K)
    top_w = jax.nn.softmax(top_val, axis=-1)
    gates = jnp.sum(jax.nn.one_hot(top_idx, N_EXPERTS, dtype=jnp.float32) * top_w[..., None], axis=-2)
    gates = gates.astype(x.dtype)
    y = gates[..., 0:1] * swiglu(x, w_gate[0], w_up[0], w_down[0])
    for e in range(1, N_EXPERTS):
        y = y + gates[..., e:e + 1] * swiglu(x, w_gate[e], w_up[e], w_down[e])
    return y


def layer(x, past, p, mix_w, ln_mix_g, ln_mix_b, ffn, w_ple_proj, w_ple_gate, ln_ffn_g, ln_ffn_b):
    m, new_rows = token_mixers(x, past, *mix_w)
    h = layer_norm(DN_ALPHA * x + m, ln_mix_g, ln_mix_b)
    ple = jax.nn.sigmoid(h @ w_ple_gate) * (p @ w_ple_proj)
    y = layer_norm(DN_ALPHA * h + ffn(h) + ple, ln_ffn_g, ln_ffn_b)
    return y, new_rows


def setup_inputs(seed: int = 0) -> dict:
    key = jax.random.key(seed)
    keys = jax.random.split(key, 40)
    counter = [0]

    def nrm(shape, scale):
        k = keys[counter[0]]
        counter[0] += 1
        return jax.random.normal(k, shape, jnp.float32) * scale

    d = D_MODEL
    return {
        'x_prompt': nrm((BATCH, SEQ, d), 1.0),
        'x_sample': nrm((DEC_BATCH, DEC_SEQ, d), 1.0),
        'cache_fox_k': nrm((DEPTH, DEC_BATCH, PAST_LEN, FOX_HEADS, FOX_HEAD_DIM), 1.0),
        'cache_fox_v': nrm((DEPTH, DEC_BATCH, PAST_LEN, FOX_HEADS, FOX_HEAD_DIM), 1.0),
        'cache_fox_logf': jax.nn.log_sigmoid(4.0 + nrm((DEPTH, DEC_BATCH, PAST_LEN, FOX_HEADS), 1.0)),
        'cache_mla_ckv': nrm((DEPTH, DEC_BATCH, PAST_LEN, MLA_KV_RANK), 1.0),
        'cache_mla_krope': nrm((DEPTH, DEC_BATCH, PAST_LEN, MLA_ROPE), 1.0),
        'p_prompt': nrm((DEPTH, BATCH, SEQ, D_PLE), 1.0),
        'p_sample': nrm((DEPTH, DEC_BATCH, DEC_SEQ, D_PLE), 1.0),
        'w_in': nrm((DEPTH, d, IN_WIDTH), d ** -0.5),
        'b_fox_f': 4.0 + nrm((DEPTH, FOX_HEADS), 0.5),
        'g_mla_cq': 1.0 + nrm((DEPTH, MLA_Q_RANK), 0.05),
        'w_mla_qb': nrm((DEPTH, MLA_Q_RANK, MLA_HEADS * (MLA_NOPE + MLA_ROPE)), MLA_Q_RANK ** -0.5),
        'g_mla_ckv': 1.0 + nrm((DEPTH, MLA_KV_RANK), 0.05),
        'w_mla_kvb': nrm((DEPTH, MLA_KV_RANK, MLA_HEADS * (MLA_NOPE + MLA_V)), MLA_KV_RANK ** -0.5),
        'w_o_fox': nrm((DEPTH, FOX_W, d), FOX_W ** -0.5),
        'w_o_mla': nrm((DEPTH, MLA_W, d), MLA_W ** -0.5),
        'w_out': nrm((DEPTH, d, d), DN_BETA * d ** -0.5),
        'ln_mix_g': 1.0 + nrm((DEPTH, d), 0.05),
        'ln_mix_b': nrm((DEPTH, d), 0.02),
        'w_ffn_gate': nrm((N_DENSE, d, D_FF), d ** -0.5),
        'w_ffn_up': nrm((N_DENSE, d, D_FF), d ** -0.5),
        'w_ffn_down': nrm((N_DENSE, D_FF, d), DN_BETA * D_FF ** -0.5),
        'w_router': nrm((N_MOE, d, N_EXPERTS), d ** -0.5),
        'b_router': nrm((N_MOE, N_EXPERTS), 0.01),
        'w_moe_gate': nrm((N_MOE, N_EXPERTS, d, D_FF_EXPERT), d ** -0.5),
        'w_moe_up': nrm((N_MOE, N_EXPERTS, d, D_FF_EXPERT), d ** -0.5),
        'w_moe_down': nrm((N_MOE, N_EXPERTS, D_FF_EXPERT, d), DN_BETA * D_FF_EXPERT ** -0.5),
        'w_ple_proj': nrm((DEPTH, D_PLE, d), DN_BETA * D_PLE ** -0.5),
        'w_ple_gate': nrm((DEPTH, d, d), d ** -0.5),
        'ln_ffn_g': 1.0 + nrm((DEPTH, d), 0.05),
        'ln_ffn_b': nrm((DEPTH, d), 0.02),
    }


def reference(x_prompt, x_sample, cache_fox_k, cache_fox_v, cache_fox_logf, cache_mla_ckv, cache_mla_krope,
              p_prompt, p_sample, w_in, b_fox_f, g_mla_cq, w_mla_qb, g_mla_ckv, w_mla_kvb, w_o_fox, w_o_mla,
              w_out, ln_mix_g, ln_mix_b, w_ffn_gate, w_ffn_up, w_ffn_down, w_router, b_router, w_moe_gate,
              w_moe_up, w_moe_down, w_ple_proj, w_ple_gate, ln_ffn_g, ln_ffn_b):
    hp, hs = x_prompt, x_sample
    rows_p, rows_s = [], []
    for i in range(DEPTH):
        mix_w = (w_in[i], b_fox_f[i], g_mla_cq[i], w_mla_qb[i], g_mla_ckv[i], w_mla_kvb[i],
                 w_o_fox[i], w_o_mla[i], w_out[i])
        j = i // 2
        if i % 2 == 0:
            ffn = functools.partial(swiglu, w_gate=w_ffn_gate[j], w_up=w_ffn_up[j], w_down=w_ffn_down[j])
        else:
            ffn = functools.partial(moe_swiglu, w_router=w_router[j], b_router=b_router[j],
                                    w_gate=w_moe_gate[j], w_up=w_moe_up[j], w_down=w_moe_down[j])
        past = (cache_fox_k[i], cache_fox_v[i], cache_fox_logf[i], cache_mla_ckv[i], cache_mla_krope[i])
        hp, new_p = layer(hp, None, p_prompt[i], mix_w, ln_mix_g[i], ln_mix_b[i], ffn,
                          w_ple_proj[i], w_ple_gate[i], ln_ffn_g[i], ln_ffn_b[i])
        hs, new_s = layer(hs, past, p_sample[i], mix_w, ln_mix_g[i], ln_mix_b[i], ffn,
                          w_ple_proj[i], w_ple_gate[i], ln_ffn_g[i], ln_ffn_b[i])
        rows_p.append(new_p)
        rows_s.append(new_s)

    def stack(rows, idx):
        return jnp.stack([r[idx] for r in rows], axis=0)

    return (hp, hs,
            stack(rows_p, 0), stack(rows_p, 1), stack(rows_p, 2), stack(rows_p, 3), stack(rows_p, 4),
            stack(rows_s, 0), stack(rows_s, 1), stack(rows_s, 2), stack(rows_s, 3), stack(rows_s, 4))
```

```python
import numpy as np
from contextlib import ExitStack
import concourse.bass as bass
import concourse.mybir as mybir
from concourse.bass_utils import run_bass_kernel_spmd

F32 = mybir.dt.float32
BF16 = mybir.dt.bfloat16
AF = mybir.ActivationFunctionType
ALU = mybir.AluOpType

D = 1024
KC = 8
NH = 8
DEPTH = 2
NE = 8
DS = 32
ALPHA = float(4.0 ** 0.25)
LN_EPS = 1e-5
RMS_EPS = 1e-6
C_FQ, C_FK, C_FV, C_FF, C_CQ, C_CKV, C_KR, C_GA, C_GB = 0, 512, 1024, 1536, 1544, 1800, 1928, 1960, 2984
SAME_ENGINE_SYNC = True
EPOCH = 30000
NSLOT = 6
SLOT_ELEMS = 2048


class Op:
    __slots__ = ("eng", "fn", "r", "w", "lane", "ndma", "deps", "sig", "tok", "lane_val", "waits", "ph")


class Prog:
    def __init__(self):
        self.ops = []
        self.phase = 'init'

    def add(self, eng, fn, r=(), w=(), lane=None, ndma=1):
        op = Op()
        op.eng = eng
        op.fn = fn
        op.r = tuple(r)
        op.w = tuple(w) + ((("lane", lane),) if lane is not None else ())
        op.lane = lane
        op.ndma = ndma
        op.sig = False
        op.tok = None
        op.ph = self.phase
        self.ops.append(op)

    def finalize(self, nc, es):
        import os
        mx_ = int(os.environ.get('KMAXOPS', '0'))
        if mx_:
            self.ops = self.ops[:mx_]
        ops = self.ops
        print('NOPS', len(ops))
        if os.environ.get('KPHASES'):
            lastp = None
            for i_, o_ in enumerate(ops):
                if o_.ph != lastp:
                    print('PHASE', i_, o_.ph)
                    lastp = o_.ph
        last_w = {}
        readers = {}
        lane_cnt = {}
        for i, op in enumerate(ops):
            deps = set()
            for k in op.r:
                j = last_w.get(k)
                if j is not None:
                    deps.add(j)
                if isinstance(k, tuple) and k[0] == "ps":
                    for j2 in readers.get(k, ()):
                        if ops[j2].eng != op.eng:
                            deps.add(j2)
            for k in op.w:
                j = last_w.get(k)
                if j is not None:
                    deps.add(j)
                rs = readers.get(k)
                if rs:
                    deps.update(rs)
            for k in op.r:
                if isinstance(k, str) and k.startswith("!"):
                    continue
                readers.setdefault(k, []).append(i)
            for k in op.w:
                last_w[k] = i
                readers[k] = []
            op.deps = deps
            if op.lane is not None:
                lane_cnt[op.lane] = lane_cnt.get(op.lane, 0) + op.ndma
                op.lane_val = 16 * lane_cnt[op.lane]
        for op in ops:
            for j in op.deps:
                pj = ops[j]
                if pj.lane is None and (pj.eng != op.eng or op.lane is not None or (SAME_ENGINE_SYNC and op.eng != 'pe')):
                    pj.sig = True
        engs = ("pe", "act", "dve", "pool", "sp")
        by_eng = {e: [] for e in engs}
        cnt = {e: 0 for e in engs}
        for op in ops:
            by_eng[op.eng].append(op)
            if op.sig:
                c = cnt[op.eng]
                op.tok = (c // EPOCH, c % EPOCH + 1)
                cnt[op.eng] = c + 1
        tok_sems = {}
        for e in engs:
            for ep in range((cnt[e] + EPOCH - 1) // EPOCH):
                tok_sems[(e, ep)] = es.enter_context(nc.semaphore(f"t_{e}_{ep}"))
        lane_sems = {ln: es.enter_context(nc.semaphore(f"l_{ln}")) for ln in lane_cnt}
        for e in engs:
            seen_tok = {}
            seen_lane = {}
            for op in by_eng[e]:
                need_tok = {}
                need_lane = {}
                for j in op.deps:
                    pj = ops[j]
                    if pj.lane is not None:
                        if pj.lane_val > need_lane.get(pj.lane, 0):
                            need_lane[pj.lane] = pj.lane_val
                    else:
                        if pj.eng == e and not (op.lane is not None or (SAME_ENGINE_SYNC and e != 'pe')):
                            continue
                        if pj.tok > need_tok.get(pj.eng, (-1, -1)):
                            need_tok[pj.eng] = pj.tok
                waits = []
                for pe_, t in need_tok.items():
                    if t > seen_tok.get(pe_, (-1, -1)):
                        seen_tok[pe_] = t
                        waits.append((tok_sems[(pe_, t[0])], t[1]))
                for ln, v in need_lane.items():
                    if v > seen_lane.get(ln, 0):
                        seen_lane[ln] = v
                        waits.append((lane_sems[ln], v))
                op.waits = waits
        block = es.enter_context(nc.Block())

        def run(ename, e):
            for op in by_eng[ename]:
                for (s, v) in op.waits:
                    e.wait_ge(s, v)
                ins = op.fn(e)
                if op.lane is not None:
                    if not isinstance(ins, (list, tuple)):
                        ins = [ins]
                    assert len(ins) == op.ndma
                    for x in ins:
                        x.then_inc(lane_sems[op.lane], 16)
                elif op.sig:
                    ins.then_inc(tok_sems[(ename, op.tok[0])], 1)
            if ename == "sp":
                for ln, c in lane_cnt.items():
                    e.wait_ge(lane_sems[ln], 16 * c)

        @block.tensor
        def _(e):
            run("pe", e)

        @block.scalar
        def _(e):
            run("act", e)

        @block.vector
        def _(e):
            run("dve", e)

        @block.gpsimd
        def _(e):
            run("pool", e)

        @block.sync
        def _(e):
            run("sp", e)
        return {e: len(by_eng[e]) for e in engs}


class Tile_:
    pass


class Seq_:
    pass


def build_program(SEQ, NP, NS, PAST, DFF):
    NT = SEQ // 128
    NB = max(1, SEQ // 512)
    BLK = min(512, SEQ)
    TT = max(SEQ, NS * DS)
    NPK = PAST // 128
    NKT = max(NT, NPK + 1)
    KTT = max(SEQ, PAST + DS)
    NFC = DFF // 128
    NPT = NKT
    QSCALE_F = float(64 ** -0.5)
    QSCALE_M = float(96 ** -0.5)

    nc = bass.Bass("TRN2", target_bir_lowering=False)
    P = Prog()
    es = ExitStack()

    def din(name, shape):
        return nc.dram_tensor(name, list(shape), F32, kind="ExternalInput").ap()

    def dout(name, shape):
        return nc.dram_tensor(name, list(shape), F32, kind="ExternalOutput").ap()

    x_p = din("x_prompt", [NP, SEQ, D])
    x_s = din("x_sample", [NS, DS, D])
    c_fk = din("cache_fox_k", [DEPTH, NS, PAST, 512])
    c_fv = din("cache_fox_v", [DEPTH, NS, PAST, 512])
    c_lf = din("cache_fox_logf", [DEPTH, NS, PAST, NH])
    c_ckv = din("cache_mla_ckv", [DEPTH, NS, PAST, 128])
    c_kr = din("cache_mla_krope", [DEPTH, NS, PAST, 32])
    p_p = din("p_prompt", [DEPTH, NP, SEQ, 256])
    p_s = din("p_sample", [DEPTH, NS, DS, 256])
    w_in = din("w_in", [DEPTH, D, 4008])
    w_qb = din("w_mla_qb", [DEPTH, 256, 768])
    w_kvb = din("w_mla_kvb", [DEPTH, 128, 1024])
    w_of = din("w_o_fox", [DEPTH, 512, D])
    w_om = din("w_o_mla", [DEPTH, 512, D])
    w_out = din("w_out", [DEPTH, D, D])
    ln_mix_g = din("ln_mix_g", [DEPTH, D])
    ln_mix_b = din("ln_mix_b", [DEPTH, D])
    w_fg = din("w_ffn_gate", [1, D, DFF])
    w_fu = din("w_ffn_up", [1, D, DFF])
    w_fd = din("w_ffn_down", [1, DFF, D])
    w_router = din("w_router", [1, D, NE])
    w_mg = din("w_moe_gate", [1, NE, D, DFF])
    w_mu = din("w_moe_up", [1, NE, D, DFF])
    w_md = din("w_moe_down", [1, NE, DFF, D])
    w_pp = din("w_ple_proj", [DEPTH, 256, D])
    w_pg = din("w_ple_gate", [DEPTH, D, D])
    ln_ffn_g = din("ln_ffn_g", [DEPTH, D])
    ln_ffn_b = din("ln_ffn_b", [DEPTH, D])
    NPAR = 16 + 512 + 256 + 8
    params = din("params", [1, NPAR])
    NCONST = 512 + 2 * NPT * 16
    consts = din("consts", [128, NCONST])

    y_p = dout("y_prompt", [NP, SEQ, D])
    y_s = dout("y_sample", [NS, DS, D])
    o_fk = {"p": dout("fox_k_prompt", [DEPTH, NP, SEQ, 512]), "s": dout("fox_k_sample", [DEPTH, NS, DS, 512])}
    o_fv = {"p": dout("fox_v_prompt", [DEPTH, NP, SEQ, 512]), "s": dout("fox_v_sample", [DEPTH, NS, DS, 512])}
    o_lf = {"p": dout("fox_logf_prompt", [DEPTH, NP, SEQ, NH]), "s": dout("fox_logf_sample", [DEPTH, NS, DS, NH])}
    o_ckv = {"p": dout("mla_ckv_prompt", [DEPTH, NP, SEQ, 128]), "s": dout("mla_ckv_sample", [DEPTH, NS, DS, 128])}
    o_kr = {"p": dout("mla_krope_prompt", [DEPTH, NP, SEQ, 32]), "s": dout("mla_krope_sample", [DEPTH, NS, DS, 32])}

    def sb(name, shape, dt):
        return es.enter_context(nc.sbuf_tensor(name, list(shape), dt))

    A = sb("A", [128, NT, D], F32)
    B = sb("B", [128, KC, TT], BF16)
    Cb = sb("C", [128, 8 * TT], BF16)
    OTF = Cb[:, 0:4 * TT].rearrange("p (c t) -> p c t", c=4)
    OTM = Cb[:, 4 * TT:8 * TT].rearrange("p (c t) -> p c t", c=4)
    ACT_ = [Cb[:, i * 2 * TT:(i + 1) * 2 * TT].rearrange("p (c t) -> p c t", c=2) for i in range(2)]
    WR = [sb(f"WR{s}", [128, SLOT_ELEMS], BF16) for s in range(NSLOT)]
    QKN = max(2 * max(TT, KTT), KC * BLK)
    QK = sb("QK", [128, QKN], BF16)
    QT = QK[:, 0:TT]
    KT = QK[:, QKN // 2:QKN // 2 + KTT]
    MGT = QK[:, 0:KC * BLK].rearrange("p (k t) -> p k t", k=KC)
    QKK = ["QT", ("KT", "lo"), ("KT", "hi")]
    VP = sb("VP", [128, NKT, 128], BF16)
    PT = [sb(f"PT{i}", [128, 512], BF16) for i in range(3)]
    CKVT = sb("CKVT", [128, KTT], BF16)
    KRR = sb("KRR", [128, NKT, 32], F32)
    LOGF = sb("LOGF", [128, NKT, NH], F32)
    CUM = sb("CUM", [128, NKT, NH], F32)
    PRE = sb("PRE", [128, NKT + 1, NH], F32)
    NQB = max(NB, 1)
    BIAS = sb("BIAS", [128, NQB, NKT, NH], F32)
    LNGB = sb("LNGB", [128, max(2 * D, TT)], F32)
    LNG = LNGB[:, 0:D]
    LNB = LNGB[:, D:2 * D]
    CQT = LNGB.bitcast(BF16)[:, 0:2 * TT].rearrange("p (c t) -> p c t", c=2)
    SC = sb("SC", [128, D], F32)
    SC2 = sb("SC2", [128, 512], F32)
    CONST = sb("CONST", [128, NCONST], F32)
    IDF = CONST[:, 0:128]
    TRIF = CONST[:, 128:256]
    ONESF = CONST[:, 256:384]
    CHKF = CONST[:, 384:512]
    COS = CONST[:, 512:512 + NPT * 16].rearrange("p (t c) -> p t c", c=16)
    SIN = CONST[:, 512 + NPT * 16:512 + 2 * NPT * 16].rearrange("p (t c) -> p t c", c=16)
    PB = sb("PB", [128, NPAR], F32)
    BFF = PB[:, 0:16].rearrange("p (l c) -> p l c", l=2)
    GCQ = PB[:, 16:528].rearrange("p (l c) -> p l c", l=2)
    GCKV = PB[:, 528:784].rearrange("p (l c) -> p l c", l=2)
    BRT = PB[:, 784:792]
    WRT = sb("WRT", [128, KC, NE], F32)
    MASKB = sb("MASKB", [128, 256], BF16)
    TRIB = MASKB[:, 0:128]
    CHKB = MASKB[:, 128:256]
    ONESB = sb("ONESB", [128, 128], BF16)
    GATES = sb("GATES", [128, NT, NE], F32)
    SM = sb("SM", [128, 64], F32)
    QS = sb("QS", [128, 4, 96], F32)
    RT = sb("RT", [128, 128], F32)
    EPSC = sb("EPSC", [128, 2], F32)
    SCK = [("SC", 0), ("SC", 1)]
    HTF = SC[:, :].rearrange("p (k t) -> p k t", k=KC)
    SC2K = [("SC2", q_) for q_ in range(4)]
    STG = [sb(f"STG{i}", [128, 256], F32) for i in range(3)]
    CKVN = [sb(f"CKVN{i}", [128, 128], F32) for i in range(2)]
    PIN = STG[0:2]
    PTT = sb("PTT", [128, 2, 128], BF16)
    ps = es.enter_context(nc.psum_tensor("ps", [128, 8, 512], F32))

    state = {"bank": 0, "abank": 0, "slot": 0, "stg": 0, "ckvn": 0, "pin": 0, "pt": 0, "xl": 0}
    slot_gen = [0] * NSLOT

    def nb():
        b = state["bank"]
        state["bank"] = (b + 1) % 8
        return b

    def nba():
        b = state["abank"]
        state["abank"] = (b + 1) % 4
        return b

    def wload(pieces, tag=""):
        s = state["slot"]
        state["slot"] = (s + 1) % NSLOT
        slot_gen[s] += 1
        views = []
        off = 0
        dmas = []
        for (src, shape) in pieces:
            n = 1
            for d_ in shape:
                n *= d_
            v = WR[s][:, off:off + n]
            if len(shape) == 2:
                v = v.rearrange("p (a b) -> p a b", a=shape[0])
            views.append(v)
            dmas.append((v, src))
            off += n
        assert off <= SLOT_ELEMS, (off, tag)

        def fn(e, dmas=dmas):
            return [e.dma_start(out=o, in_=i) for (o, i) in dmas]
        P.add("pool", fn, r=(), w=[("W", s)], lane=f"W{s}", ndma=len(dmas))
        return (s, slot_gen[s], tag), views

    def WK(tok):
        assert slot_gen[tok[0]] == tok[1], ("stale weight slot", tok)
        return ("W", tok[0])

    def win_cols(l, c0, n):
        return w_in[l].rearrange("(k p) c -> p k c", p=128)[:, :, c0:c0 + n]

    def mm_group(out, terms, r, w, start=True, stop=True):
        def fn(e, out=out, terms=terms):
            n = len(terms)
            ins = None
            for i, (lt, rh) in enumerate(terms):
                ins = e.matmul(out, lhsT=lt, rhs=rh, start=(start and i == 0), stop=(stop and i == n - 1))
            return ins
        P.add("pe", fn, r=r, w=w)

    def tr_group(items, r, w):
        def fn(e, items=items):
            ins = None
            for (o, i, rows) in items:
                ins = e.transpose(o, i, IDF[0:rows, 0:rows])
            return ins
        P.add("pe", fn, r=tuple(r) + ("!const",), w=w)

    def act_copy(out, in_, r, w):
        P.add("act", lambda e, o=out, i=in_: e.copy(o, i), r=r, w=w)

    def dve_copy(out, in_, r, w):
        P.add("dve", lambda e, o=out, i=in_: e.tensor_copy(out=o, in_=i), r=r, w=w)

    def dve_tt(out, in0, in1, op, r, w):
        P.add("dve", lambda e, o=out, a=in0, b=in1, op=op: e.tensor_tensor(out=o, in0=a, in1=b, op=op), r=r, w=w)

    def dve_ts(out, in0, s1, s2, op0, op1, r, w):
        if s2 is None:
            P.add("dve", lambda e, o=out, a=in0, s1=s1, op0=op0: e.tensor_scalar(out=o, in0=a, scalar1=s1, scalar2=None, op0=op0), r=r, w=w)
        else:
            P.add("dve", lambda e, o=out, a=in0, s1=s1, s2=s2, op0=op0, op1=op1: e.tensor_scalar(out=o, in0=a, scalar1=s1, scalar2=s2, op0=op0, op1=op1), r=r, w=w)

    def dve_stt(out, in0, scalar, in1, op0, op1, r, w):
        P.add("dve", lambda e, o=out, a=in0, s=scalar, b=in1, op0=op0, op1=op1: e.scalar_tensor_tensor(out=o, in0=a, scalar=s, in1=b, op0=op0, op1=op1), r=r, w=w)

    def act_fn(out, in_, func, r, w, bias=None, scale=None, accum=None):
        def fn(e, o=out, i=in_, func=func, bias=bias, scale=scale, accum=accum):
            kw = {}
            if bias is not None:
                kw["bias"] = bias
            if scale is not None:
                kw["scale"] = scale
            if accum is not None:
                kw["accum_out"] = accum
            return e.activation(out=o, in_=i, func=func, **kw)
        P.add("act", fn, r=r, w=w)

    def sp_dma(out, in_, r, w, lane):
        P.add("sp", lambda e, o=out, i=in_: e.dma_start(out=o, in_=i), r=r, w=w, lane=lane)

    sp_dma(CONST[:], consts[:, :], (), ["!const"], "c0")
    sp_dma(PB[:], params[0:1, :].partition_broadcast(128), (), ["!pb"], "c1")
    sp_dma(WRT[:], w_router[0].rearrange("(k p) e -> p k e", p=128), (), ["!wrt"], "c2")
    P.add("dve", lambda e: e.memset(EPSC[:, 0:1], LN_EPS), r=(), w=["!eps0"])
    P.add("dve", lambda e: e.memset(EPSC[:, 1:2], RMS_EPS), r=(), w=["!eps1"])
    dve_copy(TRIB, TRIF, ["!const"], ["!maskb"])
    dve_copy(CHKB, CHKF, ["!const"], ["!maskb2"])
    dve_copy(ONESB[:], ONESF, ["!const"], ["!onesb"])

    def make_prompt_round(i):
        R = Seq_()
        R.kind = "p"
        s = Seq_()
        s.kind = "p"
        s.idx = i
        s.n_past = 0
        s.n_new = SEQ
        s.col0 = 0
        s.tiles = []
        for t in range(NT):
            tl = Tile_()
            tl.t = t
            tl.rows = 128
            tl.col0 = 128 * t
            tl.pos0 = 128 * t
            tl.kt = t
            tl.seq = s
            tl.lpos = 128 * t
            s.tiles.append(tl)
        R.seqs = [s]
        R.tiles = list(s.tiles)
        R.blocks = [(BLK * b, BLK, R.tiles[(BLK // 128) * b:(BLK // 128) * (b + 1)]) for b in range(NB)]
        R.ncols = SEQ
        return R

    def make_sample_round():
        R = Seq_()
        R.kind = "s"
        R.seqs = []
        R.tiles = []
        for i in range(NS):
            s = Seq_()
            s.kind = "s"
            s.idx = i
            s.n_past = PAST
            s.n_new = DS
            s.col0 = DS * i
            tl = Tile_()
            tl.t = i
            tl.rows = DS
            tl.col0 = DS * i
            tl.pos0 = PAST
            tl.kt = NPK
            tl.seq = s
            tl.lpos = 0
            s.tiles = [tl]
            R.seqs.append(s)
            R.tiles.append(tl)
        R.blocks = [(0, DS * NS, list(R.tiles))]
        R.ncols = DS * NS
        return R

    def xsrc(R, tl):
        if R.kind == "p":
            return x_p[tl.seq.idx, tl.lpos:tl.lpos + tl.rows, :]
        return x_s[tl.seq.idx, :, :]

    def ysrc(R, tl):
        if R.kind == "p":
            return y_p[tl.seq.idx, tl.lpos:tl.lpos + tl.rows, :]
        return y_s[tl.seq.idx, :, :]

    def psrc(R, l, tl):
        if R.kind == "p":
            return p_p[l, tl.seq.idx, tl.lpos:tl.lpos + tl.rows, :]
        return p_s[l, tl.seq.idx, :, :]

    def rows_out(o, R, l, tl):
        return o[R.kind][l, tl.seq.idx, tl.lpos:tl.lpos + tl.rows, :]

    def tile_to_B(tl, router=False, l=0):
        n = tl.rows
        for g in range(2):
            b = nb()
            items = [(ps[:, b, 128 * i:128 * i + n], A[0:n, tl.t, 128 * (4 * g + i):128 * (4 * g + i) + 128], n) for i in range(4)]
            tr_group(items, [("A", tl.t)], [("ps", b)])
            src = ps[:, b, :].rearrange("p (i c) -> p i c", i=4)[:, :, 0:n]
            if g == 0:
                act_copy(B[:, 4 * g:4 * g + 4, tl.col0:tl.col0 + n], src, [("ps", b)], [("B", tl.col0 // 128, g)])
            else:
                dve_copy(B[:, 4 * g:4 * g + 4, tl.col0:tl.col0 + n], src, [("ps", b)], [("B", tl.col0 // 128, g)])
            if router:
                dve_copy(HTF[:, 4 * g:4 * g + 4, 0:n], src, [("ps", b)], [("HTF", g), ("SC", g)])
        if router:
            b = nb()
            terms = [(HTF[:, k, 0:n], WRT[:, k, :]) for k in range(KC)]
            mm_group(ps[0:n, b, 0:NE], terms, [("HTF", 0), ("HTF", 1), "!wrt"] + SCK, [("ps", b)])
            lg = SM[0:n, 0:8]
            dve_tt(lg, ps[0:n, b, 0:NE], BRT[0:n, :], ALU.add, [("ps", b), "!pb"], ["SMr"])
            mx = SM[0:n, 8:16]
            P.add("dve", lambda e, o=mx, i=lg: e.max(out=o, in_=i), r=["SMr"], w=["SMr2"])
            d12 = SM[0:n, 16:17]
            dve_tt(d12, SM[0:n, 8:9], SM[0:n, 9:10], ALU.subtract, ["SMr2"], ["SMr3"])
            act_fn(SM[0:n, 17:18], d12, AF.Sigmoid, ["SMr3"], ["SMr4"])
            act_fn(SM[0:n, 18:19], d12, AF.Sigmoid, ["SMr3"], ["SMr5"], scale=-1.0)
            g1 = SM[0:n, 24:32]
            dve_tt(SM[0:n, 19:20], SM[0:n, 17:18], SM[0:n, 18:19], ALU.subtract, ["SMr4", "SMr5"], ["SMr8"])
            dve_ts(g1, lg, SM[0:n, 8:9], None, ALU.is_ge, None, ["SMr", "SMr2"], ["SMr6"])
            dve_ts(g1, g1, SM[0:n, 19:20], None, ALU.mult, None, ["SMr6", "SMr8"], ["SMr6"])
            g2 = SM[0:n, 32:40]
            dve_ts(g2, lg, SM[0:n, 9:10], None, ALU.is_ge, None, ["SMr", "SMr2"], ["SMr7"])
            dve_ts(g2, g2, SM[0:n, 18:19], None, ALU.mult, None, ["SMr7", "SMr5"], ["SMr7"])
            dve_tt(GATES[0:n, tl.t, :], g1, g2, ALU.add, ["SMr6", "SMr7"], [("GATES", tl.t)])

    def Bkeys(tl):
        return [("B", tl.col0 // 128, 0), ("B", tl.col0 // 128, 1)]

    def Bkeys_cols(c0, n):
        ks = []
        for j in range(c0 // 128, (c0 + n + 127) // 128):
            ks += [("B", j, 0), ("B", j, 1)]
        return ks

    def layer_norm_tile(tl, from_A=False):
        n = tl.rows
        src = A[0:n, tl.t, :] if from_A else SC[0:n, :]
        skeys = [("A", tl.t)] if from_A else SCK
        st = SM[0:n, 40:52]
        for hh in range(2):
            P.add("dve", lambda e, o=SM[0:n, 40 + 6 * hh:46 + 6 * hh], i=src[:, 512 * hh:512 * hh + 512]: e.bn_stats(out=o, in_=i), r=([("A", tl.t)] if from_A else [("SC", hh)]), w=[("LNst", hh)])
        mv = SM[0:n, 52:54]
        P.add("dve", lambda e, o=mv, i=st: e.bn_aggr(out=o, in_=i), r=[("LNst", 0), ("LNst", 1)], w=["LNmv"])
        rstd = SM[0:n, 54:55]
        act_fn(rstd, SM[0:n, 53:54], AF.Sqrt, ["LNmv", "!eps0"], ["LNrs"], bias=EPSC[0:n, 0:1])
        P.add("dve", lambda e, o=rstd: e.reciprocal(out=o, in_=o), r=["LNrs"], w=["LNrs"])
        dve_stt(SC[0:n, :], src, SM[0:n, 52:53], LNG[0:n, :], ALU.subtract, ALU.mult, skeys + SCK + ["LNmv", "LNG"], SCK)
        dve_stt(A[0:n, tl.t, :], SC[0:n, :], rstd, LNB[0:n, :], ALU.mult, ALU.add, SCK + ["LNrs", "LNB"] + skeys, [("A", tl.t)])

    def key_blocks(s):
        kb = []
        for j in range(s.n_past // 128):
            kb.append((128 * j, 128, j))
        if s.kind == "p":
            for tl in s.tiles:
                kb.append((tl.pos0, tl.rows, tl.kt))
        else:
            kb.append((s.n_past, DS, s.n_past // 128))
        return kb

    def q_blocks(s):
        if s.kind == "p":
            return [(BLK * b, BLK, BLK * b, b) for b in range(NB)]
        return [(0, DS, s.n_past, 0)]

    def attention(s, hp, krows, scale, causal, bias_h, OT, otag, ochunk, kt_keys):
        kbs = key_blocks(s)
        lo, hi = krows
        for (q0, nq, qpos0, qb) in q_blocks(s):
            nbk, dbk = (4, 5) if (state["pt"] // 1) % 2 == 0 else (6, 7)
            state["pt"] += 1
            vis = []
            for (kpos0, nk, kt) in kbs:
                cs = max(0, kpos0 - qpos0)
                if cs >= nq:
                    continue
                if causal:
                    need_mask = (kpos0 + nk - 1) > (qpos0 + cs)
                else:
                    need_mask = ((kpos0 + nk - 1) // 64) > ((qpos0 + cs) // 64)
                w_ = min(nk, nq - cs) if need_mask else 0
                vis.append((kpos0, nk, kt, cs, w_))
            nv = len(vis)
            sbanks = [None] * nv
            ptis = [None] * nv

            def emit_qk(i):
                (kpos0, nk, kt, cs, w_) = vis[i]
                b = nba()
                sbanks[i] = b
                mm_group(ps[0:nk, b, cs:nq], [(KT[lo:hi, kpos0:kpos0 + nk], QT[lo:hi, q0 + cs:q0 + nq])],
                         ["QT"] + list(kt_keys), [("ps", b)])

            def emit_rest(i):
                (kpos0, nk, kt, cs, w_) = vis[i]
                b = sbanks[i]
                pi = i % 3
                pt = PT[pi]
                if bias_h is not None:
                    bias = BIAS[0:nk, qb, kt, bias_h:bias_h + 1]
                    rr = [("ps", b), "BIAS"]
                else:
                    bias = None
                    rr = [("ps", b)]
                act_fn(pt[0:nk, cs:nq], ps[0:nk, b, cs:nq], AF.Exp, rr, [("PT", pi)], bias=bias, scale=scale)
                if w_ > 0:
                    mk = TRIB if causal else CHKB
                    dve_tt(pt[0:nk, cs:cs + w_], pt[0:nk, cs:cs + w_], mk[0:nk, 0:w_], ALU.mult,
                           [("PT", pi), "!maskb", "!maskb2"], [("PT", pi)])
                mm_group(ps[:, nbk, cs:nq], [(VP[0:nk, kt, :], pt[0:nk, cs:nq])], [("PT", pi), "VP"], [("ps", nbk)],
                         start=(i == 0), stop=(i == nv - 1))
                mm_group(ps[:, dbk, cs:nq], [(ONESB[0:nk, :], pt[0:nk, cs:nq])], [("PT", pi), "!onesb"], [("ps", dbk)],
                         start=(i == 0), stop=(i == nv - 1))

            emit_qk(0)
            for i in range(nv):
                if i + 1 < nv:
                    emit_qk(i + 1)
                emit_rest(i)
            o0, o1 = 64 * hp, 64 * hp + 64
            rd = SC2[o0:o1, 0:nq]
            P.add("dve", lambda e, o=rd, i_=ps[o0:o1, dbk, 0:nq]: e.reciprocal(out=o, in_=i_), r=[("ps", dbk)], w=SC2K)
            dve_tt(OT[o0:o1, ochunk, s.col0 + (q0 if s.kind == "p" else 0):s.col0 + (q0 if s.kind == "p" else 0) + nq],
                   ps[o0:o1, nbk, 0:nq], rd, ALU.mult, [("ps", nbk)] + SC2K, [("OT", otag, ochunk, hp)])

    def mixer(R, l):
        P.phase = f'mixer-small {R.kind} l{l}'
        for s in R.seqs:
            s1, (wcq,) = wload([(win_cols(l, C_CQ, 256), (KC, 256))], "cq")
            s2, (wck, wff) = wload([(win_cols(l, C_CKV, 160), (KC, 160)), (win_cols(l, C_FF, 8), (KC, 8))], "ckv")
            if s.n_past:
                sp_dma(LOGF[:, 0:NPK, :], c_lf[l, s.idx].rearrange("(t p) h -> p t h", p=128), (), [("LOGF", j_) for j_ in range(NPK)], "io0")
                sp_dma(KRR[:, 0:NPK, :], c_kr[l, s.idx].rearrange("(t p) c -> p t c", p=128), (), [("KRR", j_) for j_ in range(NPK)], "io1")
            for tl in s.tiles:
                n = tl.rows
                b1 = nb()
                b2 = nb()
                cols = slice(tl.col0, tl.col0 + n)
                mm_group(ps[0:n, b1, 0:256], [(B[:, k, cols], wcq[:, k, :]) for k in range(KC)], Bkeys(tl) + [WK(s1)], [("ps", b1)])
                mm_group(ps[0:n, b2, 0:160], [(B[:, k, cols], wck[:, k, :]) for k in range(KC)], Bkeys(tl) + [WK(s2)], [("ps", b2)])
                mm_group(ps[0:n, b2, 160:168], [(B[:, k, cols], wff[:, k, :]) for k in range(KC)], Bkeys(tl) + [WK(s2)], [("ps", b2)])
                dve_tt(SM[0:n, 0:8], ps[0:n, b2, 160:168], BFF[0:n, l, :], ALU.add, [("ps", b2), "!pb"], ["SM0"])
                act_fn(SM[0:n, 8:16], SM[0:n, 0:8], AF.Exp, ["SM0"], ["SM1"], scale=-1.0)
                act_fn(SM[0:n, 16:24], SM[0:n, 8:16], AF.Ln, ["SM1"], ["SM2"], bias=1.0)
                dve_ts(LOGF[0:n, tl.kt, :], SM[0:n, 16:24], -1.0, None, ALU.mult, None, ["SM2"], [("LOGF", tl.kt)])
                act_fn(SC2[0:n, 0:256], ps[0:n, b1, 0:256], AF.Square, [("ps", b1)], [("SC2", 0), ("SC2", 1), "SMss1"], accum=SM[0:n, 24:25])
                act_fn(SM[0:n, 25:26], SM[0:n, 24:25], AF.Sqrt, ["SMss1", "!eps1"], ["SM3"], bias=EPSC[0:n, 1:2], scale=1.0 / 256)
                P.add("dve", lambda e, o=SM[0:n, 25:26]: e.reciprocal(out=o, in_=o), r=["SM3"], w=["SM3"])
                dve_stt(SC2[0:n, 0:256], ps[0:n, b1, 0:256], SM[0:n, 25:26], GCQ[0:n, l, :], ALU.mult, ALU.mult,
                        [("ps", b1), "SM3", "!pb"], [("SC2", 0), ("SC2", 1)])
                b3 = nb()
                tr_group([(ps[:, b3, 128 * c:128 * c + n], SC2[0:n, 128 * c:128 * c + 128], n) for c in range(2)],
                         [("SC2", 0), ("SC2", 1)], [("ps", b3)])
                act_copy(CQT[:, :, cols], ps[:, b3, 0:256].rearrange("p (c t) -> p c t", c=2)[:, :, 0:n], [("ps", b3)], [("CQT", tl.col0 // 128), "LNG", "LNB"])
                ci = state["ckvn"]
                state["ckvn"] = (ci + 1) % 2
                act_fn(SC2[0:n, 256:384], ps[0:n, b2, 0:128], AF.Square, [("ps", b2)], [("SC2", 2), "SMss2"], accum=SM[0:n, 26:27])
                act_fn(SM[0:n, 27:28], SM[0:n, 26:27], AF.Sqrt, ["SMss2", "!eps1"], ["SM4"], bias=EPSC[0:n, 1:2], scale=1.0 / 128)
                P.add("dve", lambda e, o=SM[0:n, 27:28]: e.reciprocal(out=o, in_=o), r=["SM4"], w=["SM4"])
                dve_stt(CKVN[ci][0:n, :], ps[0:n, b2, 0:128], SM[0:n, 27:28], GCKV[0:n, l, :], ALU.mult, ALU.mult,
                        [("ps", b2), "SM4", "!pb"], [("CKVN", ci)])
                sp_dma(rows_out(o_ckv, R, l, tl), CKVN[ci][0:n, :], [("CKVN", ci)], (), f"ck{ci}")
                b4 = nb()
                tr_group([(ps[:, b4, 0:n], CKVN[ci][0:n, :], n)], [("CKVN", ci)], [("ps", b4)])
                kc0 = s.n_past + tl.lpos
                dve_copy(CKVT[:, kc0:kc0 + n], ps[:, b4, 0:n], [("ps", b4)], [("CKVT", tl.kt)])
                x1 = ps[0:n, b2, 128:144]
                x2 = ps[0:n, b2, 144:160]
                cs_ = COS[0:n, tl.kt, :]
                sn_ = SIN[0:n, tl.kt, :]
                dve_tt(SM[0:n, 28:44], x1, cs_, ALU.mult, [("ps", b2), "!const"], ["SM5"])
                dve_tt(SM[0:n, 44:60], x2, sn_, ALU.mult, [("ps", b2), "!const"], ["SM6"])
                dve_tt(KRR[0:n, tl.kt, 0:16], SM[0:n, 28:44], SM[0:n, 44:60], ALU.subtract, ["SM5", "SM6"], [("KRR", tl.kt)])
                dve_tt(SM[0:n, 28:44], x2, cs_, ALU.mult, [("ps", b2), "!const", ("KRR", tl.kt)], ["SM5"])
                dve_tt(SM[0:n, 44:60], x1, sn_, ALU.mult, [("ps", b2), "!const", ("KRR", tl.kt)], ["SM6"])
                dve_tt(KRR[0:n, tl.kt, 16:32], SM[0:n, 28:44], SM[0:n, 44:60], ALU.add, ["SM5", "SM6"], [("KRR", tl.kt)])
            t0 = s.tiles[0]
            nt_ = len(s.tiles)
            if s.kind == "p":
                sp_dma(o_lf["p"][l, s.idx].rearrange("(t p) h -> p t h", p=128), LOGF[:, 0:nt_, :],
                       [("LOGF", tl.kt) for tl in s.tiles], (), "io2")
                sp_dma(o_kr["p"][l, s.idx].rearrange("(t p) c -> p t c", p=128), KRR[:, 0:nt_, :],
                       [("KRR", tl.kt) for tl in s.tiles], (), "io3")
            else:
                sp_dma(o_lf["s"][l, s.idx], LOGF[0:DS, t0.kt, :], [("LOGF", t0.kt)], (), "io2")
                sp_dma(o_kr["s"][l, s.idx], KRR[0:DS, t0.kt, :], [("KRR", t0.kt)], (), "io3")
            if R.kind == "s":
                seq_attention(R, l, s)
        P.phase = f'fkv-out {R.kind} l{l}'
        for (c0, o) in ((C_FK, o_fk), (C_FV, o_fv)):
            for g in range(2):
                sw, (wv,) = wload([(win_cols(l, c0 + 256 * g, 256), (KC, 256))], "fkv")
                for tl in R.tiles:
                    n = tl.rows
                    b = nb()
                    cols = slice(tl.col0, tl.col0 + n)
                    mm_group(ps[0:n, b, 0:256], [(B[:, k, cols], wv[:, k, :]) for k in range(KC)], Bkeys(tl) + [WK(sw)], [("ps", b)])
                    si = state["stg"]
                    state["stg"] = (si + 1) % 3
                    act_copy(STG[si][0:n, :], ps[0:n, b, 0:256], [("ps", b)], [("STG", si)])
                    sp_dma(rows_out(o, R, l, tl)[:, 256 * g:256 * g + 256], STG[si][0:n, :], [("STG", si)], (), f"st{si}")
        if R.kind == "p":
            seq_attention(R, l, R.seqs[0])
        P.phase = f'merge {R.kind} l{l}'
        sp_dma(LNG[:], ln_mix_g[l:l + 1, :].partition_broadcast(128), (), ["LNG"], "io4")
        sp_dma(LNB[:], ln_mix_b[l:l + 1, :].partition_broadcast(128), (), ["LNB"], "io5")
        for (c0, n_, tls) in R.blocks:
            cols = slice(c0, c0 + n_)
            for dp in range(4):
                sga, (wga,) = wload([(win_cols(l, C_GA + 256 * dp, 256), (KC, 256))], "ga")
                sgb, (wgb,) = wload([(win_cols(l, C_GB + 256 * dp, 256), (KC, 256))], "gb")
                so, (wof_, wom_) = wload([(w_of[l].rearrange("(k p) c -> p k c", p=128)[:, :, 256 * dp:256 * dp + 256], (4, 256)),
                                          (w_om[l].rearrange("(k p) c -> p k c", p=128)[:, :, 256 * dp:256 * dp + 256], (4, 256))], "wo")
                for d2 in range(2):
                    dc = 2 * dp + d2
                    ws = slice(128 * d2, 128 * d2 + 128)
                    bga, bgb, bof, bom = nb(), nb(), nb(), nb()
                    mm_group(ps[:, bga, 0:n_], [(wga[:, k, ws], B[:, k, cols]) for k in range(KC)], Bkeys_cols(c0, n_) + [WK(sga)], [("ps", bga)])
                    mm_group(ps[:, bgb, 0:n_], [(wgb[:, k, ws], B[:, k, cols]) for k in range(KC)], Bkeys_cols(c0, n_) + [WK(sgb)], [("ps", bgb)])
                    mm_group(ps[:, bof, 0:n_], [(wof_[:, k, ws], OTF[:, k, cols]) for k in range(4)],
                             [("OT", "f", k, hp) for k in range(4) for hp in range(2)] + [WK(so)], [("ps", bof)])
                    mm_group(ps[:, bom, 0:n_], [(wom_[:, k, ws], OTM[:, k, cols]) for k in range(4)],
                             [("OT", "m", k, hp) for k in range(4) for hp in range(2)] + [WK(so)], [("ps", bom)])
                    act_fn(SC[:, 0:n_], ps[:, bga, 0:n_], AF.Sigmoid, [("ps", bga)], [("SC", 0)])
                    act_fn(SC[:, 512:512 + n_], ps[:, bgb, 0:n_], AF.Sigmoid, [("ps", bgb)], [("SC", 1)])
                    dve_tt(SC[:, 0:n_], SC[:, 0:n_], ps[:, bof, 0:n_], ALU.mult, [("SC", 0), ("ps", bof)], [("SC", 0)])
                    dve_tt(SC[:, 512:512 + n_], SC[:, 512:512 + n_], ps[:, bom, 0:n_], ALU.mult, [("SC", 1), ("ps", bom)], [("SC", 1)])
                    dve_tt(MGT[:, dc, 0:n_], SC[:, 0:n_], SC[:, 512:512 + n_], ALU.add, [("SC", 0), ("SC", 1)], [("MGT", dc)] + QKK)
            for tl in tls:
                n = tl.rows
                lc = tl.col0 - c0
                bq = [nb() for _ in range(2)]
                for qd in range(4):
                    swo, (wo_,) = wload([(w_out[l].rearrange("(k p) c -> p k c", p=128)[:, :, 256 * qd:256 * qd + 256], (KC, 256))], "wout")
                    mm_group(ps[0:n, bq[qd // 2], 256 * (qd % 2):256 * (qd % 2) + 256],
                             [(MGT[:, k, lc:lc + n], wo_[:, k, :]) for k in range(KC)],
                             [("MGT", k) for k in range(KC)] + QKK + [WK(swo)], [("ps", bq[qd // 2])])
                for hh in range(2):
                    dve_stt(SC[0:n, 512 * hh:512 * hh + 512], A[0:n, tl.t, 512 * hh:512 * hh + 512], ALPHA, ps[0:n, bq[hh], :],
                            ALU.mult, ALU.add, [("A", tl.t), ("ps", bq[hh])] + SCK, SCK)
                layer_norm_tile(tl)
                tile_to_B(tl, router=(l == 1), l=l)


    def seq_attention(R, l, s):
        P.phase = f'att-cum {R.kind} l{l} s{s.idx}'
        kbs = key_blocks(s)
        qbs = q_blocks(s)
        nkt = len(kbs)
        P.add("dve", lambda e: e.memset(PRE[:, 0, :], 0.0), r=(), w=[("PRE", 0)])
        for (kpos0, nk, kt) in kbs:
            b = nb()
            lkey = ("LOGF", kt)
            mm_group(ps[:, b, 0:NH], [(ONESF[0:nk, :], LOGF[0:nk, kt, :])], [lkey, "!const"], [("ps", b)])
            mm_group(ps[0:nk, b, 8:8 + NH], [(TRIF[0:nk, 0:nk], LOGF[0:nk, kt, :])], [lkey, "!const"], [("ps", b)])
            dve_tt(PRE[:, kt + 1, :], PRE[:, kt, :], ps[:, b, 0:NH], ALU.add, [("PRE", kt), ("ps", b)], [("PRE", kt + 1)])
            dve_tt(CUM[0:nk, kt, :], PRE[0:nk, kt, :], ps[0:nk, b, 8:8 + NH], ALU.add, [("PRE", kt), ("ps", b)], [("CUM", kt)])
        for (q0, nq, qpos0, qb) in qbs:
            jq = qpos0 // 128
            P.add("dve", lambda e, o=BIAS[:, qb, 0:nkt, :], a=PRE[:, jq:jq + 1, :].to_broadcast([128, nkt, NH]), b_=CUM[:, 0:nkt, :]:
                  e.tensor_tensor(out=o, in0=a, in1=b_, op=ALU.subtract),
                  r=[("PRE", jq)] + [("CUM", kt) for (_, _, kt) in kbs], w=["BIAS"])
        npast = s.n_past
        for j in range(4):
            P.phase = f'att-fox{j} {R.kind} l{l} s{s.idx}'
            sq, (wq, wk) = wload([(win_cols(l, C_FQ + 128 * j, 128), (KC, 128)), (win_cols(l, C_FK + 128 * j, 128), (KC, 128))], "fqk")
            sv, (wv,) = wload([(win_cols(l, C_FV + 128 * j, 128), (KC, 128))], "fv")
            if npast:
                P.add("pool", lambda e, o=VP[:, 0:NPK, :], i=c_fv[l, s.idx].rearrange("(t p) c -> p t c", p=128)[:, :, 128 * j:128 * j + 128]:
                      e.dma_start(out=o, in_=i), r=(), w=["VP"], lane="vp")
                for g0 in range(0, NPK, 8):
                    g1 = min(NPK, g0 + 8)
                    sp_dma(SC[:, 0:(g1 - g0) * 128].rearrange("p (t c) -> p t c", c=128),
                           c_fk[l, s.idx].rearrange("(t p) c -> p t c", p=128)[:, g0:g1, 128 * j:128 * j + 128],
                           (), SCK, "io6")
                    for t4 in range(g0, g1, 4):
                        b = nba()
                        tr_group([(ps[:, b, 128 * i:128 * i + 128], SC[:, (t4 - g0 + i) * 128:(t4 - g0 + i) * 128 + 128], 128) for i in range(min(4, g1 - t4))],
                                 SCK, [("ps", b)])
                        nn = min(4, g1 - t4) * 128
                        act_copy(KT[:, 128 * t4:128 * t4 + nn], ps[:, b, 0:nn], [("ps", b)], [("KT", "lo"), ("KT", "hi")])
            if s.kind == "p":
                cblocks = [(c0, n_, c0, c0) for (c0, n_, _) in R.blocks]
            else:
                cblocks = [(s.col0, DS, 0, npast)]
            for (bc0, n_, qc0, kc0) in cblocks:
                b = nba()
                mm_group(ps[:, b, 0:n_], [(wq[:, k, :], B[:, k, bc0:bc0 + n_]) for k in range(KC)], Bkeys_cols(bc0, n_) + [WK(sq)], [("ps", b)])
                act_copy(QT[:, qc0:qc0 + n_], ps[:, b, 0:n_], [("ps", b)], ["QT"])
                b = nba()
                mm_group(ps[:, b, 0:n_], [(wk[:, k, :], B[:, k, bc0:bc0 + n_]) for k in range(KC)], Bkeys_cols(bc0, n_) + [WK(sq)], [("ps", b)])
                dve_copy(KT[:, kc0:kc0 + n_], ps[:, b, 0:n_], [("ps", b)], [("KT", "lo"), ("KT", "hi")])
            for g0 in range(0, len(s.tiles), 4):
                grp = s.tiles[g0:g0 + 4]
                b = nba()
                for i, tl in enumerate(grp):
                    n = tl.rows
                    mm_group(ps[0:n, b, 128 * i:128 * i + 128], [(B[:, k, tl.col0:tl.col0 + n], wv[:, k, :]) for k in range(KC)],
                             Bkeys(tl) + [WK(sv)], [("ps", b)])
                n = grp[0].rows
                act_copy(VP[0:n, grp[0].kt:grp[0].kt + len(grp), :], ps[0:n, b, 0:128 * len(grp)].rearrange("p (t c) -> p t c", c=128),
                         [("ps", b)], ["VP"])
            for hp in range(2):
                attention(s, hp, (64 * hp, 64 * hp + 64), QSCALE_F, True, 2 * j + hp, OTF, "f", j, [("KT", "lo"), ("KT", "hi")])
        P.phase = f'att-mla-prep {R.kind} l{l} s{s.idx}'
        for g0 in range(0, nkt, 4):
            grp = kbs[g0:g0 + 4]
            b = nba()
            items = []
            rk = []
            for i, (kpos0, nk, kt) in enumerate(grp):
                items.append((ps[0:32, b, 128 * i:128 * i + nk], KRR[0:nk, kt, :], nk))
                rk.append(("KRR", kt))
            tr_group(items, rk, [("ps", b)])
            if len(grp) > 1 or grp[0][1] == 128:
                full = sum(1 for g_ in grp if g_[1] == 128)
                if full:
                    dve_copy(KT[64:96, grp[0][0]:grp[0][0] + 128 * full], ps[0:32, b, 0:128 * full], [("ps", b)], [("KT", "hi")])
                for i, (kpos0, nk, kt) in enumerate(grp):
                    if nk != 128:
                        dve_copy(KT[64:96, kpos0:kpos0 + nk], ps[0:32, b, 128 * i:128 * i + nk], [("ps", b)], [("KT", "hi")])
            else:
                (kpos0, nk, kt) = grp[0]
                dve_copy(KT[64:96, kpos0:kpos0 + nk], ps[0:32, b, 0:nk], [("ps", b)], [("KT", "hi")])
        if npast:
            for g0 in range(0, NPK, 8):
                g1 = min(NPK, g0 + 8)
                sp_dma(SC[:, 0:(g1 - g0) * 128].rearrange("p (t c) -> p t c", c=128),
                       c_ckv[l, s.idx].rearrange("(t p) c -> p t c", p=128)[:, g0:g1, :], (), SCK, "io6")
                for t4 in range(g0, g1, 4):
                    b = nba()
                    cnt_ = min(4, g1 - t4)
                    tr_group([(ps[:, b, 128 * i:128 * i + 128], SC[:, (t4 - g0 + i) * 128:(t4 - g0 + i) * 128 + 128], 128) for i in range(cnt_)],
                             SCK, [("ps", b)])
                    act_copy(CKVT[:, 128 * t4:128 * t4 + 128 * cnt_], ps[:, b, 0:128 * cnt_], [("ps", b)], [("CKVT", t4 + i_) for i_ in range(cnt_)])
        ckv_keys = [("CKVT", kt_) for (_, _, kt_) in kbs]
        for h in range(NH):
            P.phase = f'att-mla{h} {R.kind} l{l} s{s.idx}'
            j, hp = h // 2, h % 2
            kvv = w_kvb[l].rearrange("p (h c) -> p h c", c=128)
            if hp == 0:
                sw, (wqh, wkh, wvp) = wload([(w_qb[l].rearrange("(k p) c -> p k c", p=128)[:, :, 96 * h:96 * h + 96], (2, 96)),
                                             (kvv[:, h, 0:64], (64,)),
                                             (kvv[:, h:h + 2, 64:128], (2, 64))], "mla")
            else:
                sw, (wqh, wkh) = wload([(w_qb[l].rearrange("(k p) c -> p k c", p=128)[:, :, 96 * h:96 * h + 96], (2, 96)),
                                        (kvv[:, h, 0:64], (64,))], "mla")
            for g0 in range(0, len(s.tiles), 4):
                grp = s.tiles[g0:g0 + 4]
                ng = len(grp)
                n = grp[0].rows
                b = nba()
                for i, tl in enumerate(grp):
                    mm_group(ps[0:n, b, 96 * i:96 * i + 96], [(CQT[:, c, tl.col0:tl.col0 + n], wqh[:, c, :]) for c in range(2)],
                             [("CQT", tl.col0 // 128), WK(sw), "LNG", "LNB"], [("ps", b)])
                pv = ps[0:n, b, 0:96 * ng].rearrange("p (t c) -> p t c", c=96)
                act_copy(QS[0:n, 0:ng, 0:64], pv[:, :, 0:64], [("ps", b)], [("QS", 0)])
                cs_ = COS[0:n, grp[0].kt:grp[0].kt + ng, :]
                sn_ = SIN[0:n, grp[0].kt:grp[0].kt + ng, :]
                T1 = RT[0:n, 0:16 * ng].rearrange("p (t c) -> p t c", c=16)
                T2 = RT[0:n, 64:64 + 16 * ng].rearrange("p (t c) -> p t c", c=16)
                dve_tt(T1, pv[:, :, 64:80], cs_, ALU.mult, [("ps", b), "!const"], [("RT", 0)])
                dve_tt(T2, pv[:, :, 80:96], sn_, ALU.mult, [("ps", b), "!const"], [("RT", 1)])
                dve_tt(QS[0:n, 0:ng, 64:80], T1, T2, ALU.subtract, [("RT", 0), ("RT", 1)], [("QS", 1)])
                dve_tt(T1, pv[:, :, 80:96], cs_, ALU.mult, [("ps", b), "!const"], [("RT", 0)])
                dve_tt(T2, pv[:, :, 64:80], sn_, ALU.mult, [("ps", b), "!const"], [("RT", 1)])
                dve_tt(QS[0:n, 0:ng, 80:96], T1, T2, ALU.add, [("RT", 0), ("RT", 1)], [("QS", 2)])
                b2 = nba()
                tr_group([(ps[0:96, b2, 128 * i:128 * i + n], QS[0:n, i, :], n) for i in range(ng)], [("QS", 0), ("QS", 1), ("QS", 2)], [("ps", b2)])
                qc0 = grp[0].col0 if s.kind == "p" else 0
                if n == 128:
                    act_copy(QT[0:96, qc0:qc0 + 128 * ng], ps[0:96, b2, 0:128 * ng], [("ps", b2)], ["QT"])
                else:
                    act_copy(QT[0:96, qc0:qc0 + n], ps[0:96, b2, 0:n], [("ps", b2)], ["QT"])
            ktot = npast + s.n_new
            for c0 in range(0, ktot, 512):
                n_ = min(512, ktot - c0)
                b = nba()
                mm_group(ps[0:64, b, 0:n_], [(wkh, CKVT[:, c0:c0 + n_])], ckv_keys + [WK(sw)], [("ps", b)])
                dve_copy(KT[0:64, c0:c0 + n_], ps[0:64, b, 0:n_], [("ps", b)], [("KT", "lo")])
            if hp == 0:
                for g0 in range(0, nkt, 4):
                    grp = kbs[g0:g0 + 4]
                    b = nba()
                    for i, (kpos0, nk, kt) in enumerate(grp):
                        mm_group(ps[0:nk, b, 128 * i:128 * i + 128], [(CKVT[:, kpos0:kpos0 + nk], wvp.rearrange("p a b -> p (a b)"))],
                                 ckv_keys + [WK(sw)], [("ps", b)])
                    for i, (kpos0, nk, kt) in enumerate(grp):
                        if i == 0 or nk != 128:
                            cntf = sum(1 for g_ in grp if g_[1] == 128) if (i == 0 and nk == 128) else 1
                            act_copy(VP[0:nk, kt:kt + cntf, :], ps[0:nk, b, 128 * i:128 * (i + cntf)].rearrange("p (t c) -> p t c", c=128),
                                     [("ps", b)], ["VP"])
            attention(s, hp, (0, 96), QSCALE_M, False, None, OTM, "m", j, [("KT", "lo"), ("KT", "hi")])

    def ffn(R, l, last):
        P.phase = f'ffn-ple {R.kind} l{l}'
        sp_dma(LNG[:], ln_ffn_g[l:l + 1, :].partition_broadcast(128), (), ["LNG"], "io4")
        sp_dma(LNB[:], ln_ffn_b[l:l + 1, :].partition_broadcast(128), (), ["LNB"], "io5")
        for qd in range(4):
            spg, (wpg_,) = wload([(w_pg[l].rearrange("(k p) c -> p k c", p=128)[:, :, 256 * qd:256 * qd + 256], (KC, 256))], "wpg")
            spp, (wpp_,) = wload([(w_pp[l].rearrange("(k p) c -> p k c", p=128)[:, :, 256 * qd:256 * qd + 256], (2, 256))], "wpp")
            for tl in R.tiles:
                n = tl.rows
                cols = slice(tl.col0, tl.col0 + n)
                pi = state["pin"]
                state["pin"] = (pi + 1) % 2
                sp_dma(PIN[pi][0:n, :], psrc(R, l, tl), (), [("STG", pi)], f"st{pi}")
                b = nb()
                tr_group([(ps[:, b, 128 * c:128 * c + n], PIN[pi][0:n, 128 * c:128 * c + 128], n) for c in range(2)], [("STG", pi)], [("ps", b)])
                act_copy(PTT[:, :, 0:n], ps[:, b, 0:256].rearrange("p (c t) -> p c t", c=2)[:, :, 0:n], [("ps", b)], ["PTT"])
                bg = nb()
                mm_group(ps[0:n, bg, 0:256], [(B[:, k, cols], wpg_[:, k, :]) for k in range(KC)], Bkeys(tl) + [WK(spg)], [("ps", bg)])
                mm_group(ps[0:n, bg, 256:512], [(PTT[:, c, 0:n], wpp_[:, c, :]) for c in range(2)], ["PTT", WK(spp)], [("ps", bg)])
                act_fn(SC2[0:n, 0:256], ps[0:n, bg, 0:256], AF.Sigmoid, [("ps", bg)], [("SC2", 0), ("SC2", 1)])
                dve_tt(SC2[0:n, 0:256], SC2[0:n, 0:256], ps[0:n, bg, 256:512], ALU.mult, [("SC2", 0), ("ps", bg)], [("SC2", 0), ("SC2", 1)])
                asl = A[0:n, tl.t, 256 * qd:256 * qd + 256]
                dve_stt(asl, asl, ALPHA, SC2[0:n, 0:256], ALU.mult, ALU.add, [("A", tl.t), ("SC2", 0)], [("A", tl.t)])
        P.phase = f'ffn-swiglu {R.kind} l{l}'
        experts = [None] if l % 2 == 0 else list(range(NE))
        gi = 0
        pending = []

        def flush(k):
            for _ in range(min(k, len(pending))):
                pending.pop(0)()

        for ex in experts:
            if ex is None:
                wg_d = w_fg[0].rearrange("(k p) f -> p k f", p=128)
                wu_d = w_fu[0].rearrange("(k p) f -> p k f", p=128)
                wd_d = w_fd[0].rearrange("(c p) d -> p c d", p=128)
            else:
                wg_d = w_mg[0, ex].rearrange("(k p) f -> p k f", p=128)
                wu_d = w_mu[0, ex].rearrange("(k p) f -> p k f", p=128)
                wd_d = w_md[0, ex].rearrange("(c p) d -> p c d", p=128)
            for f0 in range(0, NFC, 2):
                nf = min(2, NFC - f0)
                sg_, (wg_,) = wload([(wg_d[:, :, 128 * f0:128 * (f0 + nf)], (KC, 128 * nf))], "wg")
                su_, (wu_,) = wload([(wu_d[:, :, 128 * f0:128 * (f0 + nf)], (KC, 128 * nf))], "wu")
                sd_, (wd_,) = wload([(wd_d[:, f0:f0 + nf, :], (nf, D))], "wd")
                AT = ACT_[gi % 2]
                ai = gi % 2
                gi += 1
                g_iters = [(fl, blk) for fl in range(nf) for blk in R.blocks]
                per = (len(pending) + len(g_iters) - 1) // len(g_iters)
                for (fl, (c0, n_, tls)) in g_iters:
                    ws = slice(128 * fl, 128 * fl + 128)
                    cols = slice(c0, c0 + n_)
                    bg, bu = nb(), nb()
                    mm_group(ps[:, bg, 0:n_], [(wg_[:, k, ws], B[:, k, cols]) for k in range(KC)], Bkeys_cols(c0, n_) + [WK(sg_)], [("ps", bg)])
                    mm_group(ps[:, bu, 0:n_], [(wu_[:, k, ws], B[:, k, cols]) for k in range(KC)], Bkeys_cols(c0, n_) + [WK(su_)], [("ps", bu)])
                    act_fn(SC[:, 0:n_], ps[:, bg, 0:n_], AF.Silu, [("ps", bg)], [("SC", 0)])
                    dve_tt(AT[:, fl, cols], SC[:, 0:n_], ps[:, bu, 0:n_], ALU.mult, [("SC", 0), ("ps", bu)], [("ACT", ai, fl, c0)])
                    flush(per)
                flush(len(pending))

                def d_bank(tl, hh, AT=AT, ai=ai, nf=nf, wd_=wd_, sd_=sd_, ex=ex):
                    n = tl.rows
                    bc0 = [c0 for (c0, n_, _) in R.blocks if c0 <= tl.col0 < c0 + n_][0]
                    b = nb()
                    mm_group(ps[0:n, b, :], [(AT[:, fl, tl.col0:tl.col0 + n], wd_[:, fl, 512 * hh:512 * hh + 512]) for fl in range(nf)],
                             [("ACT", ai, fl, bc0) for fl in range(nf)] + [WK(sd_)], [("ps", b)])
                    asl = A[0:n, tl.t, 512 * hh:512 * hh + 512]
                    if ex is None:
                        dve_tt(asl, asl, ps[0:n, b, :], ALU.add, [("A", tl.t), ("ps", b)], [("A", tl.t)])
                    else:
                        dve_stt(asl, ps[0:n, b, :], GATES[0:n, tl.t, ex:ex + 1], asl, ALU.mult, ALU.add,
                                [("A", tl.t), ("ps", b), ("GATES", tl.t)], [("A", tl.t)])
                for tl in R.tiles:
                    for hh in range(2):
                        pending.append(lambda tl=tl, hh=hh, f_=d_bank: f_(tl, hh))
        flush(len(pending))
        P.phase = f'ln2 {R.kind} l{l}'
        for tl in R.tiles:
            n = tl.rows
            layer_norm_tile(tl, from_A=True)
            if last:
                sp_dma(ysrc(R, tl), A[0:n, tl.t, :], [("A", tl.t)], (), f"y{tl.t % 4}")
            else:
                tile_to_B(tl)

    rounds = [make_prompt_round(i) for i in range(NP)] + ([make_sample_round()] if NS else [])
    for R in rounds:
        for tl in R.tiles:
            xl = state["xl"]
            state["xl"] = (xl + 1) % 4
            sp_dma(A[0:tl.rows, tl.t, :], xsrc(R, tl), (), [("A", tl.t)], f"x{xl}")
            tile_to_B(tl)
        for l in range(DEPTH):
            mixer(R, l)
            ffn(R, l, last=(l == DEPTH - 1))

    counts = P.finalize(nc, es)
    es.close()
    return nc, counts


def make_consts(NPT):
    c = np.zeros((128, 512 + 2 * NPT * 16), np.float32)
    c[:, 0:128] = np.eye(128, dtype=np.float32)
    i = np.arange(128)
    c[:, 128:256] = (i[:, None] <= i[None, :]).astype(np.float32)
    c[:, 256:384] = 1.0
    c[:, 384:512] = ((i[:, None] // 64) <= (i[None, :] // 64)).astype(np.float32)
    half = 16
    inv = (np.float32(10000.0) ** (-np.arange(half, dtype=np.float32) * np.float32(2.0) / np.float32(32))).astype(np.float32)
    pos = np.arange(NPT * 128, dtype=np.float32)
    ang = (pos[:, None] * inv[None, :]).astype(np.float32)
    cos = np.cos(ang).astype(np.float32).reshape(NPT, 128, 16).transpose(1, 0, 2).reshape(128, NPT * 16)
    sin = np.sin(ang).astype(np.float32).reshape(NPT, 128, 16).transpose(1, 0, 2).reshape(128, NPT * 16)
    c[:, 512:512 + NPT * 16] = cos
    c[:, 512 + NPT * 16:] = sin
    return c


_CACHE = {}


def run(inputs, n_cores):
    x_prompt = np.asarray(inputs["x_prompt"])
    x_sample = np.asarray(inputs["x_sample"])
    BATCH, SEQ, _ = x_prompt.shape
    DECB, _, _ = x_sample.shape
    PAST = inputs["cache_fox_k"].shape[2]
    DFF = inputs["w_ffn_gate"].shape[2]
    NP = BATCH // n_cores
    NS = DECB // n_cores
    key = (SEQ, NP, NS, PAST, DFF)
    if key not in _CACHE:
        _CACHE[key] = build_program(*key)
    nc, counts = _CACHE[key]
    NKT = max(SEQ // 128, PAST // 128 + 1)
    consts = make_consts(NKT)
    params = np.concatenate([np.asarray(inputs["b_fox_f"], np.float32).ravel(), np.asarray(inputs["g_mla_cq"], np.float32).ravel(),
                             np.asarray(inputs["g_mla_ckv"], np.float32).ravel(), np.asarray(inputs["b_router"], np.float32).ravel()])[None, :]
    shared = {k: np.ascontiguousarray(np.asarray(inputs[k], np.float32)) for k in
              ("w_in", "w_mla_qb", "w_mla_kvb", "w_o_fox", "w_o_mla", "w_out", "ln_mix_g", "ln_mix_b", "w_ffn_gate", "w_ffn_up",
               "w_ffn_down", "w_router", "w_moe_gate", "w_moe_up", "w_moe_down", "w_ple_proj", "w_ple_gate", "ln_ffn_g", "ln_ffn_b")}
    shared["params"] = np.ascontiguousarray(params)
    shared["consts"] = consts
    in_maps = []
    for c in range(n_cores):
        m = dict(shared)
        ps_, ss_ = slice(c * NP, (c + 1) * NP), slice(c * NS, (c + 1) * NS)
        m["x_prompt"] = np.ascontiguousarray(x_prompt[ps_])
        m["x_sample"] = np.ascontiguousarray(x_sample[ss_])
        m["cache_fox_k"] = np.ascontiguousarray(np.asarray(inputs["cache_fox_k"])[:, ss_].reshape(DEPTH, NS, PAST, 512))
        m["cache_fox_v"] = np.ascontiguousarray(np.asarray(inputs["cache_fox_v"])[:, ss_].reshape(DEPTH, NS, PAST, 512))
        m["cache_fox_logf"] = np.ascontiguousarray(np.asarray(inputs["cache_fox_logf"])[:, ss_])
        m["cache_mla_ckv"] = np.ascontiguousarray(np.asarray(inputs["cache_mla_ckv"])[:, ss_])
        m["cache_mla_krope"] = np.ascontiguousarray(np.asarray(inputs["cache_mla_krope"])[:, ss_])
        m["p_prompt"] = np.ascontiguousarray(np.asarray(inputs["p_prompt"])[:, ps_])
        m["p_sample"] = np.ascontiguousarray(np.asarray(inputs["p_sample"])[:, ss_])
        in_maps.append(m)
    res = run_bass_kernel_spmd(nc, in_maps, core_ids=list(range(n_cores)))
    rs = res.results

    def cat(name, axis, shape_tail=None):
        a = np.concatenate([np.asarray(r[name]) for r in rs], axis=axis)
        return a

    y_p = cat("y_prompt", 0)
    y_s = cat("y_sample", 0)
    outs = [y_p, y_s]
    for sfx, nb_, sl in (("prompt", BATCH, SEQ), ("sample", DECB, DS)):
        fk = cat(f"fox_k_{sfx}", 1).reshape(DEPTH, nb_, sl, NH, 64)
        fv = cat(f"fox_v_{sfx}", 1).reshape(DEPTH, nb_, sl, NH, 64)
        lf = cat(f"fox_logf_{sfx}", 1)
        ck = cat(f"mla_ckv_{sfx}", 1)
        kr = cat(f"mla_krope_{sfx}", 1)
        outs += [fk, fv, lf, ck, kr]
    return tuple(np.ascontiguousarray(o.astype(np.float32)) for o in outs)


def kernel(**inputs):
    return run(inputs, 8)
```

```python
import numpy as np
from contextlib import ExitStack
import concourse.bass as bass
import concourse.mybir as mybir
from concourse.bass_utils import run_bass_kernel_spmd

F32 = mybir.dt.float32
BF16 = mybir.dt.bfloat16
AF = mybir.ActivationFunctionType
ALU = mybir.AluOpType

D = 1024
KC = 8
NH = 8
DEPTH = 2
NE = 8
DS = 32
ALPHA = float(4.0 ** 0.25)
LN_EPS = 1e-5
RMS_EPS = 1e-6
C_FQ, C_FK, C_FV, C_FF, C_CQ, C_CKV, C_KR, C_GA, C_GB = 0, 512, 1024, 1536, 1544, 1800, 1928, 1960, 2984
SAME_ENGINE_SYNC = True
EPOCH = 30000
NSLOT = 6
SLOT_ELEMS = 2048


class Op:
    __slots__ = ("eng", "fn", "r", "w", "lane", "ndma", "deps", "sig", "tok", "lane_val", "waits", "ph")


class Prog:
    def __init__(self):
        self.ops = []
        self.phase = 'init'

    def add(self, eng, fn, r=(), w=(), lane=None, ndma=1):
        op = Op()
        op.eng = eng
        op.fn = fn
        op.r = tuple(r)
        op.w = tuple(w) + ((("lane", lane),) if lane is not None else ())
        op.lane = lane
        op.ndma = ndma
        op.sig = False
        op.tok = None
        op.ph = self.phase
        self.ops.append(op)

    def finalize(self, nc, es):
        import os
        mx_ = int(os.environ.get('KMAXOPS', '0'))
        if mx_:
            self.ops = self.ops[:mx_]
        ops = self.ops
        print('NOPS', len(ops))
        if os.environ.get('KPHASES'):
            lastp = None
            for i_, o_ in enumerate(ops):
                if o_.ph != lastp:
                    print('PHASE', i_, o_.ph)
                    lastp = o_.ph
        last_w = {}
        readers = {}
        lane_cnt = {}
        for i, op in enumerate(ops):
            deps = set()
            for k in op.r:
                j = last_w.get(k)
                if j is not None:
                    deps.add(j)
                if isinstance(k, tuple) and k[0] == "ps":
                    for j2 in readers.get(k, ()):
                        if ops[j2].eng != op.eng:
                            deps.add(j2)
            for k in op.w:
                j = last_w.get(k)
                if j is not None:
                    deps.add(j)
                rs = readers.get(k)
                if rs:
                    deps.update(rs)
            for k in op.r:
                if isinstance(k, str) and k.startswith("!"):
                    continue
                readers.setdefault(k, []).append(i)
            for k in op.w:
                last_w[k] = i
                readers[k] = []
            op.deps = deps
            if op.lane is not None:
                lane_cnt[op.lane] = lane_cnt.get(op.lane, 0) + op.ndma
                op.lane_val = 16 * lane_cnt[op.lane]
        for op in ops:
            for j in op.deps:
                pj = ops[j]
                if pj.lane is None and (pj.eng != op.eng or op.lane is not None or (SAME_ENGINE_SYNC and op.eng != 'pe')):
                    pj.sig = True
        engs = ("pe", "act", "dve", "pool", "sp")
        by_eng = {e: [] for e in engs}
        cnt = {e: 0 for e in engs}
        for op in ops:
            by_eng[op.eng].append(op)
            if op.sig:
                c = cnt[op.eng]
                op.tok = (c // EPOCH, c % EPOCH + 1)
                cnt[op.eng] = c + 1
        tok_sems = {}
        for e in engs:
            for ep in range((cnt[e] + EPOCH - 1) // EPOCH):
                tok_sems[(e, ep)] = es.enter_context(nc.semaphore(f"t_{e}_{ep}"))
        lane_sems = {ln: es.enter_context(nc.semaphore(f"l_{ln}")) for ln in lane_cnt}
        for e in engs:
            seen_tok = {}
            seen_lane = {}
            for op in by_eng[e]:
                need_tok = {}
                need_lane = {}
                for j in op.deps:
                    pj = ops[j]
                    if pj.lane is not None:
                        if pj.lane_val > need_lane.get(pj.lane, 0):
                            need_lane[pj.lane] = pj.lane_val
                    else:
                        if pj.eng == e and not (op.lane is not None or (SAME_ENGINE_SYNC and e != 'pe')):
                            continue
                        if pj.tok > need_tok.get(pj.eng, (-1, -1)):
                            need_tok[pj.eng] = pj.tok
                waits = []
                for pe_, t in need_tok.items():
                    if t > seen_tok.get(pe_, (-1, -1)):
                        seen_tok[pe_] = t
                        waits.append((tok_sems[(pe_, t[0])], t[1]))
                for ln, v in need_lane.items():
                    if v > seen_lane.get(ln, 0):
                        seen_lane[ln] = v
                        waits.append((lane_sems[ln], v))
                op.waits = waits
        block = es.enter_context(nc.Block())

        def run(ename, e):
            for op in by_eng[ename]:
                for (s, v) in op.waits:
                    e.wait_ge(s, v)
                ins = op.fn(e)
                if op.lane is not None:
                    if not isinstance(ins, (list, tuple)):
                        ins = [ins]
                    assert len(ins) == op.ndma
                    for x in ins:
                        x.then_inc(lane_sems[op.lane], 16)
                elif op.sig:
                    ins.then_inc(tok_sems[(ename, op.tok[0])], 1)
            if ename == "sp":
                for ln, c in lane_cnt.items():
                    e.wait_ge(lane_sems[ln], 16 * c)

        @block.tensor
        def _(e):
            run("pe", e)

        @block.scalar
        def _(e):
            run("act", e)

        @block.vector
        def _(e):
            run("dve", e)

        @block.gpsimd
        def _(e):
            run("pool", e)

        @block.sync
        def _(e):
            run("sp", e)
        return {e: len(by_eng[e]) for e in engs}


class Tile_:
    pass


class Seq_:
    pass


def build_program(SEQ, NP, NS, PAST, DFF):
    NT = SEQ // 128
    NB = max(1, SEQ // 512)
    BLK = min(512, SEQ)
    TT = max(SEQ, NS * DS)
    NPK = PAST // 128
    NKT = max(NT, NPK + 1)
    KTT = max(SEQ, PAST + DS)
    NFC = DFF // 128
    NPT = NKT
    QSCALE_F = float(64 ** -0.5)
    QSCALE_M = float(96 ** -0.5)

    nc = bass.Bass("TRN2", target_bir_lowering=False)
    P = Prog()
    es = ExitStack()

    def din(name, shape):
        return nc.dram_tensor(name, list(shape), F32, kind="ExternalInput").ap()

    def dout(name, shape):
        return nc.dram_tensor(name, list(shape), F32, kind="ExternalOutput").ap()

    x_p = din("x_prompt", [NP, SEQ, D])
    x_s = din("x_sample", [NS, DS, D])
    c_fk = din("cache_fox_k", [DEPTH, NS, PAST, 512])
    c_fv = din("cache_fox_v", [DEPTH, NS, PAST, 512])
    c_lf = din("cache_fox_logf", [DEPTH, NS, PAST, NH])
    c_ckv = din("cache_mla_ckv", [DEPTH, NS, PAST, 128])
    c_kr = din("cache_mla_krope", [DEPTH, NS, PAST, 32])
    p_p = din("p_prompt", [DEPTH, NP, SEQ, 256])
    p_s = din("p_sample", [DEPTH, NS, DS, 256])
    w_in = din("w_in", [DEPTH, D, 4008])
    w_qb = din("w_mla_qb", [DEPTH, 256, 768])
    w_kvb = din("w_mla_kvb", [DEPTH, 128, 1024])
    w_of = din("w_o_fox", [DEPTH, 512, D])
    w_om = din("w_o_mla", [DEPTH, 512, D])
    w_out = din("w_out", [DEPTH, D, D])
    ln_mix_g = din("ln_mix_g", [DEPTH, D])
    ln_mix_b = din("ln_mix_b", [DEPTH, D])
    w_fg = din("w_ffn_gate", [1, D, DFF])
    w_fu = din("w_ffn_up", [1, D, DFF])
    w_fd = din("w_ffn_down", [1, DFF, D])
    w_router = din("w_router", [1, D, NE])
    w_mg = din("w_moe_gate", [1, NE, D, DFF])
    w_mu = din("w_moe_up", [1, NE, D, DFF])
    w_md = din("w_moe_down", [1, NE, DFF, D])
    w_pp = din("w_ple_proj", [DEPTH, 256, D])
    w_pg = din("w_ple_gate", [DEPTH, D, D])
    ln_ffn_g = din("ln_ffn_g", [DEPTH, D])
    ln_ffn_b = din("ln_ffn_b", [DEPTH, D])
    NPAR = 16 + 512 + 256 + 8
    params = din("params", [1, NPAR])
    NCONST = 512 + 2 * NPT * 16
    consts = din("consts", [128, NCONST])

    y_p = dout("y_prompt", [NP, SEQ, D])
    y_s = dout("y_sample", [NS, DS, D])
    o_fk = {"p": dout("fox_k_prompt", [DEPTH, NP, SEQ, 512]), "s": dout("fox_k_sample", [DEPTH, NS, DS, 512])}
    o_fv = {"p": dout("fox_v_prompt", [DEPTH, NP, SEQ, 512]), "s": dout("fox_v_sample", [DEPTH, NS, DS, 512])}
    o_lf = {"p": dout("fox_logf_prompt", [DEPTH, NP, SEQ, NH]), "s": dout("fox_logf_sample", [DEPTH, NS, DS, NH])}
    o_ckv = {"p": dout("mla_ckv_prompt", [DEPTH, NP, SEQ, 128]), "s": dout("mla_ckv_sample", [DEPTH, NS, DS, 128])}
    o_kr = {"p": dout("mla_krope_prompt", [DEPTH, NP, SEQ, 32]), "s": dout("mla_krope_sample", [DEPTH, NS, DS, 32])}

    def sb(name, shape, dt):
        return es.enter_context(nc.sbuf_tensor(name, list(shape), dt))

    A = sb("A", [128, NT, D], F32)
    B = sb("B", [128, KC, TT], BF16)
    Cb = sb("C", [128, 8 * TT], BF16)
    OTF = Cb[:, 0:4 * TT].rearrange("p (c t) -> p c t", c=4)
    OTM = Cb[:, 4 * TT:8 * TT].rearrange("p (c t) -> p c t", c=4)
    ACT_ = [Cb[:, i * 2 * TT:(i + 1) * 2 * TT].rearrange("p (c t) -> p c t", c=2) for i in range(2)]
    WR = [sb(f"WR{s}", [128, SLOT_ELEMS], BF16) for s in range(NSLOT)]
    QKN = max(2 * max(TT, KTT), KC * BLK)
    QK = sb("QK", [128, QKN], BF16)
    QT = QK[:, 0:TT]
    KT = QK[:, QKN // 2:QKN // 2 + KTT]
    MGT = QK[:, 0:KC * BLK].rearrange("p (k t) -> p k t", k=KC)
    QKK = ["QT", ("KT", "lo"), ("KT", "hi")]
    VP = sb("VP", [128, NKT, 128], BF16)
    PT = [sb(f"PT{i}", [128, 512], BF16) for i in range(3)]
    CKVT = sb("CKVT", [128, KTT], BF16)
    KRR = sb("KRR", [128, NKT, 32], F32)
    LOGF = sb("LOGF", [128, NKT, NH], F32)
    CUM = sb("CUM", [128, NKT, NH], F32)
    PRE = sb("PRE", [128, NKT + 1, NH], F32)
    NQB = max(NB, 1)
    BIAS = sb("BIAS", [128, NQB, NKT, NH], F32)
    LNGB = sb("LNGB", [128, max(2 * D, TT)], F32)
    LNG = LNGB[:, 0:D]
    LNB = LNGB[:, D:2 * D]
    CQT = LNGB.bitcast(BF16)[:, 0:2 * TT].rearrange("p (c t) -> p c t", c=2)
    SC = sb("SC", [128, D], F32)
    SC2 = sb("SC2", [128, 512], F32)
    CONST = sb("CONST", [128, NCONST], F32)
    IDF = CONST[:, 0:128]
    TRIF = CONST[:, 128:256]
    ONESF = CONST[:, 256:384]
    CHKF = CONST[:, 384:512]
    COS = CONST[:, 512:512 + NPT * 16].rearrange("p (t c) -> p t c", c=16)
    SIN = CONST[:, 512 + NPT * 16:512 + 2 * NPT * 16].rearrange("p (t c) -> p t c", c=16)
    PB = sb("PB", [128, NPAR], F32)
    BFF = PB[:, 0:16].rearrange("p (l c) -> p l c", l=2)
    GCQ = PB[:, 16:528].rearrange("p (l c) -> p l c", l=2)
    GCKV = PB[:, 528:784].rearrange("p (l c) -> p l c", l=2)
    BRT = PB[:, 784:792]
    WRT = sb("WRT", [128, KC, NE], F32)
    MASKB = sb("MASKB", [128, 256], BF16)
    TRIB = MASKB[:, 0:128]
    CHKB = MASKB[:, 128:256]
    ONESB = sb("ONESB", [128, 128], BF16)
    GATES = sb("GATES", [128, NT, NE], F32)
    SM = sb("SM", [128, 64], F32)
    QS = sb("QS", [128, 4, 96], F32)
    RT = sb("RT", [128, 128], F32)
    EPSC = sb("EPSC", [128, 2], F32)
    SCK = [("SC", 0), ("SC", 1)]
    HTF = SC[:, :].rearrange("p (k t) -> p k t", k=KC)
    SC2K = [("SC2", q_) for q_ in range(4)]
    STG = [sb(f"STG{i}", [128, 256], F32) for i in range(3)]
    CKVN = [sb(f"CKVN{i}", [128, 128], F32) for i in range(2)]
    PIN = STG[0:2]
    PTT = sb("PTT", [128, 2, 128], BF16)
    ps = es.enter_context(nc.psum_tensor("ps", [128, 8, 512], F32))

    state = {"bank": 0, "abank": 0, "slot": 0, "stg": 0, "ckvn": 0, "pin": 0, "pt": 0, "xl": 0}
    slot_gen = [0] * NSLOT

    def nb():
        b = state["bank"]
        state["bank"] = (b + 1) % 8
        return b

    def nba():
        b = state["abank"]
        state["abank"] = (b + 1) % 4
        return b

    def wload(pieces, tag=""):
        s = state["slot"]
        state["slot"] = (s + 1) % NSLOT
        slot_gen[s] += 1
        views = []
        off = 0
        dmas = []
        for (src, shape) in pieces:
            n = 1
            for d_ in shape:
                n *= d_
            v = WR[s][:, off:off + n]
            if len(shape) == 2:
                v = v.rearrange("p (a b) -> p a b", a=shape[0])
            views.append(v)
            dmas.append((v, src))
            off += n
        assert off <= SLOT_ELEMS, (off, tag)

        def fn(e, dmas=dmas):
            return [e.dma_start(out=o, in_=i) for (o, i) in dmas]
        P.add("pool", fn, r=(), w=[("W", s)], lane=f"W{s}", ndma=len(dmas))
        return (s, slot_gen[s], tag), views

    def WK(tok):
        assert slot_gen[tok[0]] == tok[1], ("stale weight slot", tok)
        return ("W", tok[0])

    def win_cols(l, c0, n):
        return w_in[l].rearrange("(k p) c -> p k c", p=128)[:, :, c0:c0 + n]

    def mm_group(out, terms, r, w, start=True, stop=True):
        def fn(e, out=out, terms=terms):
            n = len(terms)
            ins = None
            for i, (lt, rh) in enumerate(terms):
                ins = e.matmul(out, lhsT=lt, rhs=rh, start=(start and i == 0), stop=(stop and i == n - 1))
            return ins
        P.add("pe", fn, r=r, w=w)

    def tr_group(items, r, w):
        def fn(e, items=items):
            ins = None
            for (o, i, rows) in items:
                ins = e.transpose(o, i, IDF[0:rows, 0:rows])
            return ins
        P.add("pe", fn, r=tuple(r) + ("!const",), w=w)

    def act_copy(out, in_, r, w):
        P.add("act", lambda e, o=out, i=in_: e.copy(o, i), r=r, w=w)

    def dve_copy(out, in_, r, w):
        P.add("dve", lambda e, o=out, i=in_: e.tensor_copy(out=o, in_=i), r=r, w=w)

    def dve_tt(out, in0, in1, op, r, w):
        P.add("dve", lambda e, o=out, a=in0, b=in1, op=op: e.tensor_tensor(out=o, in0=a, in1=b, op=op), r=r, w=w)

    def dve_ts(out, in0, s1, s2, op0, op1, r, w):
        if s2 is None:
            P.add("dve", lambda e, o=out, a=in0, s1=s1, op0=op0: e.tensor_scalar(out=o, in0=a, scalar1=s1, scalar2=None, op0=op0), r=r, w=w)
        else:
            P.add("dve", lambda e, o=out, a=in0, s1=s1, s2=s2, op0=op0, op1=op1: e.tensor_scalar(out=o, in0=a, scalar1=s1, scalar2=s2, op0=op0, op1=op1), r=r, w=w)

    def dve_stt(out, in0, scalar, in1, op0, op1, r, w):
        P.add("dve", lambda e, o=out, a=in0, s=scalar, b=in1, op0=op0, op1=op1: e.scalar_tensor_tensor(out=o, in0=a, scalar=s, in1=b, op0=op0, op1=op1), r=r, w=w)

    def act_fn(out, in_, func, r, w, bias=None, scale=None, accum=None):
        def fn(e, o=out, i=in_, func=func, bias=bias, scale=scale, accum=accum):
            kw = {}
            if bias is not None:
                kw["bias"] = bias
            if scale is not None:
                kw["scale"] = scale
            if accum is not None:
                kw["accum_out"] = accum
            return e.activation(out=o, in_=i, func=func, **kw)
        P.add("act", fn, r=r, w=w)

    def sp_dma(out, in_, r, w, lane):
        P.add("sp", lambda e, o=out, i=in_: e.dma_start(out=o, in_=i), r=r, w=w, lane=lane)

    sp_dma(CONST[:], consts[:, :], (), ["!const"], "c0")
    sp_dma(PB[:], params[0:1, :].partition_broadcast(128), (), ["!pb"], "c1")
    sp_dma(WRT[:], w_router[0].rearrange("(k p) e -> p k e", p=128), (), ["!wrt"], "c2")
    P.add("dve", lambda e: e.memset(EPSC[:, 0:1], LN_EPS), r=(), w=["!eps0"])
    P.add("dve", lambda e: e.memset(EPSC[:, 1:2], RMS_EPS), r=(), w=["!eps1"])
    dve_copy(TRIB, TRIF, ["!const"], ["!maskb"])
    dve_copy(CHKB, CHKF, ["!const"], ["!maskb2"])
    dve_copy(ONESB[:], ONESF, ["!const"], ["!onesb"])

    def make_prompt_round(i):
        R = Seq_()
        R.kind = "p"
        s = Seq_()
        s.kind = "p"
        s.idx = i
        s.n_past = 0
        s.n_new = SEQ
        s.col0 = 0
        s.tiles = []
        for t in range(NT):
            tl = Tile_()
            tl.t = t
            tl.rows = 128
            tl.col0 = 128 * t
            tl.pos0 = 128 * t
            tl.kt = t
            tl.seq = s
            tl.lpos = 128 * t
            s.tiles.append(tl)
        R.seqs = [s]
        R.tiles = list(s.tiles)
        R.blocks = [(BLK * b, BLK, R.tiles[(BLK // 128) * b:(BLK // 128) * (b + 1)]) for b in range(NB)]
        R.ncols = SEQ
        return R

    def make_sample_round():
        R = Seq_()
        R.kind = "s"
        R.seqs = []
        R.tiles = []
        for i in range(NS):
            s = Seq_()
            s.kind = "s"
            s.idx = i
            s.n_past = PAST
            s.n_new = DS
            s.col0 = DS * i
            tl = Tile_()
            tl.t = i
            tl.rows = DS
            tl.col0 = DS * i
            tl.pos0 = PAST
            tl.kt = NPK
            tl.seq = s
            tl.lpos = 0
            s.tiles = [tl]
            R.seqs.append(s)
            R.tiles.append(tl)
        R.blocks = [(0, DS * NS, list(R.tiles))]
        R.ncols = DS * NS
        return R

    def xsrc(R, tl):
        if R.kind == "p":
            return x_p[tl.seq.idx, tl.lpos:tl.lpos + tl.rows, :]
        return x_s[tl.seq.idx, :, :]

    def ysrc(R, tl):
        if R.kind == "p":
            return y_p[tl.seq.idx, tl.lpos:tl.lpos + tl.rows, :]
        return y_s[tl.seq.idx, :, :]

    def psrc(R, l, tl):
        if R.kind == "p":
            return p_p[l, tl.seq.idx, tl.lpos:tl.lpos + tl.rows, :]
        return p_s[l, tl.seq.idx, :, :]

    def rows_out(o, R, l, tl):
        return o[R.kind][l, tl.seq.idx, tl.lpos:tl.lpos + tl.rows, :]

    def tile_to_B(tl, router=False, l=0):
        n = tl.rows
        for g in range(2):
            b = nb()
            items = [(ps[:, b, 128 * i:128 * i + n], A[0:n, tl.t, 128 * (4 * g + i):128 * (4 * g + i) + 128], n) for i in range(4)]
            tr_group(items, [("A", tl.t)], [("ps", b)])
            src = ps[:, b, :].rearrange("p (i c) -> p i c", i=4)[:, :, 0:n]
            if g == 0:
                act_copy(B[:, 4 * g:4 * g + 4, tl.col0:tl.col0 + n], src, [("ps", b)], [("B", tl.col0 // 128, g)])
            else:
                dve_copy(B[:, 4 * g:4 * g + 4, tl.col0:tl.col0 + n], src, [("ps", b)], [("B", tl.col0 // 128, g)])
            if router:
                dve_copy(HTF[:, 4 * g:4 * g + 4, 0:n], src, [("ps", b)], [("HTF", g), ("SC", g)])
        if router:
            b = nb()
            terms = [(HTF[:, k, 0:n], WRT[:, k, :]) for k in range(KC)]
            mm_group(ps[0:n, b, 0:NE], terms, [("HTF", 0), ("HTF", 1), "!wrt"] + SCK, [("ps", b)])
            lg = SM[0:n, 0:8]
            dve_tt(lg, ps[0:n, b, 0:NE], BRT[0:n, :], ALU.add, [("ps", b), "!pb"], ["SMr"])
            mx = SM[0:n, 8:16]
            P.add("dve", lambda e, o=mx, i=lg: e.max(out=o, in_=i), r=["SMr"], w=["SMr2"])
            d12 = SM[0:n, 16:17]
            dve_tt(d12, SM[0:n, 8:9], SM[0:n, 9:10], ALU.subtract, ["SMr2"], ["SMr3"])
            act_fn(SM[0:n, 17:18], d12, AF.Sigmoid, ["SMr3"], ["SMr4"])
            act_fn(SM[0:n, 18:19], d12, AF.Sigmoid, ["SMr3"], ["SMr5"], scale=-1.0)
            g1 = SM[0:n, 24:32]
            dve_tt(SM[0:n, 19:20], SM[0:n, 17:18], SM[0:n, 18:19], ALU.subtract, ["SMr4", "SMr5"], ["SMr8"])
            dve_ts(g1, lg, SM[0:n, 8:9], None, ALU.is_ge, None, ["SMr", "SMr2"], ["SMr6"])
            dve_ts(g1, g1, SM[0:n, 19:20], None, ALU.mult, None, ["SMr6", "SMr8"], ["SMr6"])
            g2 = SM[0:n, 32:40]
            dve_ts(g2, lg, SM[0:n, 9:10], None, ALU.is_ge, None, ["SMr", "SMr2"], ["SMr7"])
            dve_ts(g2, g2, SM[0:n, 18:19], None, ALU.mult, None, ["SMr7", "SMr5"], ["SMr7"])
            dve_tt(GATES[0:n, tl.t, :], g1, g2, ALU.add, ["SMr6", "SMr7"], [("GATES", tl.t)])

    def Bkeys(tl):
        return [("B", tl.col0 // 128, 0), ("B", tl.col0 // 128, 1)]

    def Bkeys_cols(c0, n):
        ks = []
        for j in range(c0 // 128, (c0 + n + 127) // 128):
            ks += [("B", j, 0), ("B", j, 1)]
        return ks

    def layer_norm_tile(tl, from_A=False):
        n = tl.rows
        src = A[0:n, tl.t, :] if from_A else SC[0:n, :]
        skeys = [("A", tl.t)] if from_A else SCK
        st = SM[0:n, 40:52]
        for hh in range(2):
            P.add("dve", lambda e, o=SM[0:n, 40 + 6 * hh:46 + 6 * hh], i=src[:, 512 * hh:512 * hh + 512]: e.bn_stats(out=o, in_=i), r=([("A", tl.t)] if from_A else [("SC", hh)]), w=[("LNst", hh)])
        mv = SM[0:n, 52:54]
        P.add("dve", lambda e, o=mv, i=st: e.bn_aggr(out=o, in_=i), r=[("LNst", 0), ("LNst", 1)], w=["LNmv"])
        rstd = SM[0:n, 54:55]
        act_fn(rstd, SM[0:n, 53:54], AF.Sqrt, ["LNmv", "!eps0"], ["LNrs"], bias=EPSC[0:n, 0:1])
        P.add("dve", lambda e, o=rstd: e.reciprocal(out=o, in_=o), r=["LNrs"], w=["LNrs"])
        dve_stt(SC[0:n, :], src, SM[0:n, 52:53], LNG[0:n, :], ALU.subtract, ALU.mult, skeys + SCK + ["LNmv", "LNG"], SCK)
        dve_stt(A[0:n, tl.t, :], SC[0:n, :], rstd, LNB[0:n, :], ALU.mult, ALU.add, SCK + ["LNrs", "LNB"] + skeys, [("A", tl.t)])

    def key_blocks(s):
        kb = []
        for j in range(s.n_past // 128):
            kb.append((128 * j, 128, j))
        if s.kind == "p":
            for tl in s.tiles:
                kb.append((tl.pos0, tl.rows, tl.kt))
        else:
            kb.append((s.n_past, DS, s.n_past // 128))
        return kb

    def q_blocks(s):
        if s.kind == "p":
            return [(BLK * b, BLK, BLK * b, b) for b in range(NB)]
        return [(0, DS, s.n_past, 0)]

    def attention(s, hp, krows, scale, causal, bias_h, OT, otag, ochunk, kt_keys):
        kbs = key_blocks(s)
        lo, hi = krows
        for (q0, nq, qpos0, qb) in q_blocks(s):
            nbk, dbk = (4, 5) if (state["pt"] // 1) % 2 == 0 else (6, 7)
            state["pt"] += 1
            vis = []
            for (kpos0, nk, kt) in kbs:
                cs = max(0, kpos0 - qpos0)
                if cs >= nq:
                    continue
                if causal:
                    need_mask = (kpos0 + nk - 1) > (qpos0 + cs)
                else:
                    need_mask = ((kpos0 + nk - 1) // 64) > ((qpos0 + cs) // 64)
                w_ = min(nk, nq - cs) if need_mask else 0
                vis.append((kpos0, nk, kt, cs, w_))
            nv = len(vis)
            sbanks = [None] * nv
            ptis = [None] * nv

            def emit_qk(i):
                (kpos0, nk, kt, cs, w_) = vis[i]
                b = nba()
                sbanks[i] = b
                mm_group(ps[0:nk, b, cs:nq], [(KT[lo:hi, kpos0:kpos0 + nk], QT[lo:hi, q0 + cs:q0 + nq])],
                         ["QT"] + list(kt_keys), [("ps", b)])

            def emit_rest(i):
                (kpos0, nk, kt, cs, w_) = vis[i]
                b = sbanks[i]
                pi = i % 3
                pt = PT[pi]
                if bias_h is not None:
                    bias = BIAS[0:nk, qb, kt, bias_h:bias_h + 1]
                    rr = [("ps", b), "BIAS"]
                else:
                    bias = None
                    rr = [("ps", b)]
                act_fn(pt[0:nk, cs:nq], ps[0:nk, b, cs:nq], AF.Exp, rr, [("PT", pi)], bias=bias, scale=scale)
                if w_ > 0:
                    mk = TRIB if causal else CHKB
                    dve_tt(pt[0:nk, cs:cs + w_], pt[0:nk, cs:cs + w_], mk[0:nk, 0:w_], ALU.mult,
                           [("PT", pi), "!maskb", "!maskb2"], [("PT", pi)])
                mm_group(ps[:, nbk, cs:nq], [(VP[0:nk, kt, :], pt[0:nk, cs:nq])], [("PT", pi), "VP"], [("ps", nbk)],
                         start=(i == 0), stop=(i == nv - 1))
                mm_group(ps[:, dbk, cs:nq], [(ONESB[0:nk, :], pt[0:nk, cs:nq])], [("PT", pi), "!onesb"], [("ps", dbk)],
                         start=(i == 0), stop=(i == nv - 1))

            LOOK = 2
            for i in range(min(LOOK, nv)):
                emit_qk(i)
            for i in range(nv):
                if i + LOOK < nv:
                    emit_qk(i + LOOK)
                emit_rest(i)
            o0, o1 = 64 * hp, 64 * hp + 64
            rd = SC2[o0:o1, 0:nq]
            P.add("dve", lambda e, o=rd, i_=ps[o0:o1, dbk, 0:nq]: e.reciprocal(out=o, in_=i_), r=[("ps", dbk)], w=SC2K)
            dve_tt(OT[o0:o1, ochunk, s.col0 + (q0 if s.kind == "p" else 0):s.col0 + (q0 if s.kind == "p" else 0) + nq],
                   ps[o0:o1, nbk, 0:nq], rd, ALU.mult, [("ps", nbk)] + SC2K, [("OT", otag, ochunk, hp)])

    def mixer(R, l):
        P.phase = f'mixer-small {R.kind} l{l}'
        for s in R.seqs:
            s1, (wcq,) = wload([(win_cols(l, C_CQ, 256), (KC, 256))], "cq")
            s2, (wck, wff) = wload([(win_cols(l, C_CKV, 160), (KC, 160)), (win_cols(l, C_FF, 8), (KC, 8))], "ckv")
            if s.n_past:
                sp_dma(LOGF[:, 0:NPK, :], c_lf[l, s.idx].rearrange("(t p) h -> p t h", p=128), (), [("LOGF", j_) for j_ in range(NPK)], "io0")
                sp_dma(KRR[:, 0:NPK, :], c_kr[l, s.idx].rearrange("(t p) c -> p t c", p=128), (), [("KRR", j_) for j_ in range(NPK)], "io1")
            for tl in s.tiles:
                n = tl.rows
                b1 = nb()
                b2 = nb()
                cols = slice(tl.col0, tl.col0 + n)
                mm_group(ps[0:n, b1, 0:256], [(B[:, k, cols], wcq[:, k, :]) for k in range(KC)], Bkeys(tl) + [WK(s1)], [("ps", b1)])
                mm_group(ps[0:n, b2, 0:160], [(B[:, k, cols], wck[:, k, :]) for k in range(KC)], Bkeys(tl) + [WK(s2)], [("ps", b2)])
                mm_group(ps[0:n, b2, 160:168], [(B[:, k, cols], wff[:, k, :]) for k in range(KC)], Bkeys(tl) + [WK(s2)], [("ps", b2)])
                dve_tt(SM[0:n, 0:8], ps[0:n, b2, 160:168], BFF[0:n, l, :], ALU.add, [("ps", b2), "!pb"], ["SM0"])
                act_fn(SM[0:n, 8:16], SM[0:n, 0:8], AF.Exp, ["SM0"], ["SM1"], scale=-1.0)
                act_fn(SM[0:n, 16:24], SM[0:n, 8:16], AF.Ln, ["SM1"], ["SM2"], bias=1.0)
                dve_ts(LOGF[0:n, tl.kt, :], SM[0:n, 16:24], -1.0, None, ALU.mult, None, ["SM2"], [("LOGF", tl.kt)])
                act_fn(SC2[0:n, 0:256], ps[0:n, b1, 0:256], AF.Square, [("ps", b1)], [("SC2", 0), ("SC2", 1), "SMss1"], accum=SM[0:n, 24:25])
                act_fn(SM[0:n, 25:26], SM[0:n, 24:25], AF.Sqrt, ["SMss1", "!eps1"], ["SM3"], bias=EPSC[0:n, 1:2], scale=1.0 / 256)
                P.add("dve", lambda e, o=SM[0:n, 25:26]: e.reciprocal(out=o, in_=o), r=["SM3"], w=["SM3"])
                dve_stt(SC2[0:n, 0:256], ps[0:n, b1, 0:256], SM[0:n, 25:26], GCQ[0:n, l, :], ALU.mult, ALU.mult,
                        [("ps", b1), "SM3", "!pb"], [("SC2", 0), ("SC2", 1)])
                b3 = nb()
                tr_group([(ps[:, b3, 128 * c:128 * c + n], SC2[0:n, 128 * c:128 * c + 128], n) for c in range(2)],
                         [("SC2", 0), ("SC2", 1)], [("ps", b3)])
                act_copy(CQT[:, :, cols], ps[:, b3, 0:256].rearrange("p (c t) -> p c t", c=2)[:, :, 0:n], [("ps", b3)], [("CQT", tl.col0 // 128), "LNG", "LNB"])
                ci = state["ckvn"]
                state["ckvn"] = (ci + 1) % 2
                act_fn(SC2[0:n, 256:384], ps[0:n, b2, 0:128], AF.Square, [("ps", b2)], [("SC2", 2), "SMss2"], accum=SM[0:n, 26:27])
                act_fn(SM[0:n, 27:28], SM[0:n, 26:27], AF.Sqrt, ["SMss2", "!eps1"], ["SM4"], bias=EPSC[0:n, 1:2], scale=1.0 / 128)
                P.add("dve", lambda e, o=SM[0:n, 27:28]: e.reciprocal(out=o, in_=o), r=["SM4"], w=["SM4"])
                dve_stt(CKVN[ci][0:n, :], ps[0:n, b2, 0:128], SM[0:n, 27:28], GCKV[0:n, l, :], ALU.mult, ALU.mult,
                        [("ps", b2), "SM4", "!pb"], [("CKVN", ci)])
                sp_dma(rows_out(o_ckv, R, l, tl), CKVN[ci][0:n, :], [("CKVN", ci)], (), f"ck{ci}")
                b4 = nb()
                tr_group([(ps[:, b4, 0:n], CKVN[ci][0:n, :], n)], [("CKVN", ci)], [("ps", b4)])
                kc0 = s.n_past + tl.lpos
                dve_copy(CKVT[:, kc0:kc0 + n], ps[:, b4, 0:n], [("ps", b4)], [("CKVT", tl.kt)])
                x1 = ps[0:n, b2, 128:144]
                x2 = ps[0:n, b2, 144:160]
                cs_ = COS[0:n, tl.kt, :]
                sn_ = SIN[0:n, tl.kt, :]
                dve_tt(SM[0:n, 28:44], x1, cs_, ALU.mult, [("ps", b2), "!const"], ["SM5"])
                dve_tt(SM[0:n, 44:60], x2, sn_, ALU.mult, [("ps", b2), "!const"], ["SM6"])
                dve_tt(KRR[0:n, tl.kt, 0:16], SM[0:n, 28:44], SM[0:n, 44:60], ALU.subtract, ["SM5", "SM6"], [("KRR", tl.kt)])
                dve_tt(SM[0:n, 28:44], x2, cs_, ALU.mult, [("ps", b2), "!const", ("KRR", tl.kt)], ["SM5"])
                dve_tt(SM[0:n, 44:60], x1, sn_, ALU.mult, [("ps", b2), "!const", ("KRR", tl.kt)], ["SM6"])
                dve_tt(KRR[0:n, tl.kt, 16:32], SM[0:n, 28:44], SM[0:n, 44:60], ALU.add, ["SM5", "SM6"], [("KRR", tl.kt)])
            t0 = s.tiles[0]
            nt_ = len(s.tiles)
            if s.kind == "p":
                sp_dma(o_lf["p"][l, s.idx].rearrange("(t p) h -> p t h", p=128), LOGF[:, 0:nt_, :],
                       [("LOGF", tl.kt) for tl in s.tiles], (), "io2")
                sp_dma(o_kr["p"][l, s.idx].rearrange("(t p) c -> p t c", p=128), KRR[:, 0:nt_, :],
                       [("KRR", tl.kt) for tl in s.tiles], (), "io3")
            else:
                sp_dma(o_lf["s"][l, s.idx], LOGF[0:DS, t0.kt, :], [("LOGF", t0.kt)], (), "io2")
                sp_dma(o_kr["s"][l, s.idx], KRR[0:DS, t0.kt, :], [("KRR", t0.kt)], (), "io3")
            if R.kind == "s":
                seq_attention(R, l, s)
        P.phase = f'fkv-out {R.kind} l{l}'
        for (c0, o) in ((C_FK, o_fk), (C_FV, o_fv)):
            for g in range(2):
                sw, (wv,) = wload([(win_cols(l, c0 + 256 * g, 256), (KC, 256))], "fkv")
                for tl in R.tiles:
                    n = tl.rows
                    b = nb()
                    cols = slice(tl.col0, tl.col0 + n)
                    mm_group(ps[0:n, b, 0:256], [(B[:, k, cols], wv[:, k, :]) for k in range(KC)], Bkeys(tl) + [WK(sw)], [("ps", b)])
                    si = state["stg"]
                    state["stg"] = (si + 1) % 3
                    act_copy(STG[si][0:n, :], ps[0:n, b, 0:256], [("ps", b)], [("STG", si)])
                    sp_dma(rows_out(o, R, l, tl)[:, 256 * g:256 * g + 256], STG[si][0:n, :], [("STG", si)], (), f"st{si}")
        if R.kind == "p":
            seq_attention(R, l, R.seqs[0])
        P.phase = f'merge {R.kind} l{l}'
        sp_dma(LNG[:], ln_mix_g[l:l + 1, :].partition_broadcast(128), (), ["LNG"], "io4")
        sp_dma(LNB[:], ln_mix_b[l:l + 1, :].partition_broadcast(128), (), ["LNB"], "io5")
        for (c0, n_, tls) in R.blocks:
            cols = slice(c0, c0 + n_)
            for dp in range(4):
                sga, (wga,) = wload([(win_cols(l, C_GA + 256 * dp, 256), (KC, 256))], "ga")
                sgb, (wgb,) = wload([(win_cols(l, C_GB + 256 * dp, 256), (KC, 256))], "gb")
                so, (wof_, wom_) = wload([(w_of[l].rearrange("(k p) c -> p k c", p=128)[:, :, 256 * dp:256 * dp + 256], (4, 256)),
                                          (w_om[l].rearrange("(k p) c -> p k c", p=128)[:, :, 256 * dp:256 * dp + 256], (4, 256))], "wo")
                for d2 in range(2):
                    dc = 2 * dp + d2
                    ws = slice(128 * d2, 128 * d2 + 128)
                    bga, bgb, bof, bom = nb(), nb(), nb(), nb()
                    mm_group(ps[:, bga, 0:n_], [(wga[:, k, ws], B[:, k, cols]) for k in range(KC)], Bkeys_cols(c0, n_) + [WK(sga)], [("ps", bga)])
                    mm_group(ps[:, bgb, 0:n_], [(wgb[:, k, ws], B[:, k, cols]) for k in range(KC)], Bkeys_cols(c0, n_) + [WK(sgb)], [("ps", bgb)])
                    mm_group(ps[:, bof, 0:n_], [(wof_[:, k, ws], OTF[:, k, cols]) for k in range(4)],
                             [("OT", "f", k, hp) for k in range(4) for hp in range(2)] + [WK(so)], [("ps", bof)])
                    mm_group(ps[:, bom, 0:n_], [(wom_[:, k, ws], OTM[:, k, cols]) for k in range(4)],
                             [("OT", "m", k, hp) for k in range(4) for hp in range(2)] + [WK(so)], [("ps", bom)])
                    act_fn(SC[:, 0:n_], ps[:, bga, 0:n_], AF.Sigmoid, [("ps", bga)], [("SC", 0)])
                    act_fn(SC[:, 512:512 + n_], ps[:, bgb, 0:n_], AF.Sigmoid, [("ps", bgb)], [("SC", 1)])
                    dve_tt(SC[:, 0:n_], SC[:, 0:n_], ps[:, bof, 0:n_], ALU.mult, [("SC", 0), ("ps", bof)], [("SC", 0)])
                    dve_tt(SC[:, 512:512 + n_], SC[:, 512:512 + n_], ps[:, bom, 0:n_], ALU.mult, [("SC", 1), ("ps", bom)], [("SC", 1)])
                    dve_tt(MGT[:, dc, 0:n_], SC[:, 0:n_], SC[:, 512:512 + n_], ALU.add, [("SC", 0), ("SC", 1)], [("MGT", dc)] + QKK)
            wos = []
            for qd in range(4):
                swo, (wo_,) = wload([(w_out[l].rearrange("(k p) c -> p k c", p=128)[:, :, 256 * qd:256 * qd + 256], (KC, 256))], "wout")
                wos.append((swo, wo_))
            for tl in tls:
                n = tl.rows
                lc = tl.col0 - c0
                bq = [nb() for _ in range(2)]
                for qd in range(4):
                    swo, wo_ = wos[qd]
                    mm_group(ps[0:n, bq[qd // 2], 256 * (qd % 2):256 * (qd % 2) + 256],
                             [(MGT[:, k, lc:lc + n], wo_[:, k, :]) for k in range(KC)],
                             [("MGT", k) for k in range(KC)] + QKK + [WK(swo)], [("ps", bq[qd // 2])])
                for hh in range(2):
                    dve_stt(SC[0:n, 512 * hh:512 * hh + 512], A[0:n, tl.t, 512 * hh:512 * hh + 512], ALPHA, ps[0:n, bq[hh], :],
                            ALU.mult, ALU.add, [("A", tl.t), ("ps", bq[hh])] + SCK, SCK)
                layer_norm_tile(tl)
                tile_to_B(tl, router=(l == 1), l=l)


    def seq_attention(R, l, s):
        P.phase = f'att-cum {R.kind} l{l} s{s.idx}'
        kbs = key_blocks(s)
        qbs = q_blocks(s)
        nkt = len(kbs)
        P.add("dve", lambda e: e.memset(PRE[:, 0, :], 0.0), r=(), w=[("PRE", 0)])
        for (kpos0, nk, kt) in kbs:
            b = nb()
            lkey = ("LOGF", kt)
            mm_group(ps[:, b, 0:NH], [(ONESF[0:nk, :], LOGF[0:nk, kt, :])], [lkey, "!const"], [("ps", b)])
            mm_group(ps[0:nk, b, 8:8 + NH], [(TRIF[0:nk, 0:nk], LOGF[0:nk, kt, :])], [lkey, "!const"], [("ps", b)])
            dve_tt(PRE[:, kt + 1, :], PRE[:, kt, :], ps[:, b, 0:NH], ALU.add, [("PRE", kt), ("ps", b)], [("PRE", kt + 1)])
            dve_tt(CUM[0:nk, kt, :], PRE[0:nk, kt, :], ps[0:nk, b, 8:8 + NH], ALU.add, [("PRE", kt), ("ps", b)], [("CUM", kt)])
        for (q0, nq, qpos0, qb) in qbs:
            jq = qpos0 // 128
            P.add("dve", lambda e, o=BIAS[:, qb, 0:nkt, :], a=PRE[:, jq:jq + 1, :].to_broadcast([128, nkt, NH]), b_=CUM[:, 0:nkt, :]:
                  e.tensor_tensor(out=o, in0=a, in1=b_, op=ALU.subtract),
                  r=[("PRE", jq)] + [("CUM", kt) for (_, _, kt) in kbs], w=["BIAS"])
        npast = s.n_past
        for j in range(4):
            P.phase = f'att-fox{j} {R.kind} l{l} s{s.idx}'
            sq, (wq, wk) = wload([(win_cols(l, C_FQ + 128 * j, 128), (KC, 128)), (win_cols(l, C_FK + 128 * j, 128), (KC, 128))], "fqk")
            sv, (wv,) = wload([(win_cols(l, C_FV + 128 * j, 128), (KC, 128))], "fv")
            if npast:
                P.add("pool", lambda e, o=VP[:, 0:NPK, :], i=c_fv[l, s.idx].rearrange("(t p) c -> p t c", p=128)[:, :, 128 * j:128 * j + 128]:
                      e.dma_start(out=o, in_=i), r=(), w=["VP"], lane="vp")
                for g0 in range(0, NPK, 8):
                    g1 = min(NPK, g0 + 8)
                    sp_dma(SC[:, 0:(g1 - g0) * 128].rearrange("p (t c) -> p t c", c=128),
                           c_fk[l, s.idx].rearrange("(t p) c -> p t c", p=128)[:, g0:g1, 128 * j:128 * j + 128],
                           (), SCK, "io6")
                    for t4 in range(g0, g1, 4):
                        b = nba()
                        tr_group([(ps[:, b, 128 * i:128 * i + 128], SC[:, (t4 - g0 + i) * 128:(t4 - g0 + i) * 128 + 128], 128) for i in range(min(4, g1 - t4))],
                                 SCK, [("ps", b)])
                        nn = min(4, g1 - t4) * 128
                        act_copy(KT[:, 128 * t4:128 * t4 + nn], ps[:, b, 0:nn], [("ps", b)], [("KT", "lo"), ("KT", "hi")])
            if s.kind == "p":
                cblocks = [(c0, n_, c0, c0) for (c0, n_, _) in R.blocks]
            else:
                cblocks = [(s.col0, DS, 0, npast)]
            for (bc0, n_, qc0, kc0) in cblocks:
                b = nba()
                mm_group(ps[:, b, 0:n_], [(wq[:, k, :], B[:, k, bc0:bc0 + n_]) for k in range(KC)], Bkeys_cols(bc0, n_) + [WK(sq)], [("ps", b)])
                act_copy(QT[:, qc0:qc0 + n_], ps[:, b, 0:n_], [("ps", b)], ["QT"])
                b = nba()
                mm_group(ps[:, b, 0:n_], [(wk[:, k, :], B[:, k, bc0:bc0 + n_]) for k in range(KC)], Bkeys_cols(bc0, n_) + [WK(sq)], [("ps", b)])
                dve_copy(KT[:, kc0:kc0 + n_], ps[:, b, 0:n_], [("ps", b)], [("KT", "lo"), ("KT", "hi")])
            for g0 in range(0, len(s.tiles), 4):
                grp = s.tiles[g0:g0 + 4]
                b = nba()
                for i, tl in enumerate(grp):
                    n = tl.rows
                    mm_group(ps[0:n, b, 128 * i:128 * i + 128], [(B[:, k, tl.col0:tl.col0 + n], wv[:, k, :]) for k in range(KC)],
                             Bkeys(tl) + [WK(sv)], [("ps", b)])
                n = grp[0].rows
                act_copy(VP[0:n, grp[0].kt:grp[0].kt + len(grp), :], ps[0:n, b, 0:128 * len(grp)].rearrange("p (t c) -> p t c", c=128),
                         [("ps", b)], ["VP"])
            for hp in range(2):
                attention(s, hp, (64 * hp, 64 * hp + 64), QSCALE_F, True, 2 * j + hp, OTF, "f", j, [("KT", "lo"), ("KT", "hi")])
        P.phase = f'att-mla-prep {R.kind} l{l} s{s.idx}'
        for g0 in range(0, nkt, 4):
            grp = kbs[g0:g0 + 4]
            b = nba()
            items = []
            rk = []
            for i, (kpos0, nk, kt) in enumerate(grp):
                items.append((ps[0:32, b, 128 * i:128 * i + nk], KRR[0:nk, kt, :], nk))
                rk.append(("KRR", kt))
            tr_group(items, rk, [("ps", b)])
            if len(grp) > 1 or grp[0][1] == 128:
                full = sum(1 for g_ in grp if g_[1] == 128)
                if full:
                    dve_copy(KT[64:96, grp[0][0]:grp[0][0] + 128 * full], ps[0:32, b, 0:128 * full], [("ps", b)], [("KT", "hi")])
                for i, (kpos0, nk, kt) in enumerate(grp):
                    if nk != 128:
                        dve_copy(KT[64:96, kpos0:kpos0 + nk], ps[0:32, b, 128 * i:128 * i + nk], [("ps", b)], [("KT", "hi")])
            else:
                (kpos0, nk, kt) = grp[0]
                dve_copy(KT[64:96, kpos0:kpos0 + nk], ps[0:32, b, 0:nk], [("ps", b)], [("KT", "hi")])
        if npast:
            for g0 in range(0, NPK, 8):
                g1 = min(NPK, g0 + 8)
                sp_dma(SC[:, 0:(g1 - g0) * 128].rearrange("p (t c) -> p t c", c=128),
                       c_ckv[l, s.idx].rearrange("(t p) c -> p t c", p=128)[:, g0:g1, :], (), SCK, "io6")
                for t4 in range(g0, g1, 4):
                    b = nba()
                    cnt_ = min(4, g1 - t4)
                    tr_group([(ps[:, b, 128 * i:128 * i + 128], SC[:, (t4 - g0 + i) * 128:(t4 - g0 + i) * 128 + 128], 128) for i in range(cnt_)],
                             SCK, [("ps", b)])
                    act_copy(CKVT[:, 128 * t4:128 * t4 + 128 * cnt_], ps[:, b, 0:128 * cnt_], [("ps", b)], [("CKVT", t4 + i_) for i_ in range(cnt_)])
        ckv_keys = [("CKVT", kt_) for (_, _, kt_) in kbs]
        for h in range(NH):
            P.phase = f'att-mla{h} {R.kind} l{l} s{s.idx}'
            j, hp = h // 2, h % 2
            kvv = w_kvb[l].rearrange("p (h c) -> p h c", c=128)
            if hp == 0:
                sw, (wqh, wkh, wvp) = wload([(w_qb[l].rearrange("(k p) c -> p k c", p=128)[:, :, 96 * h:96 * h + 96], (2, 96)),
                                             (kvv[:, h, 0:64], (64,)),
                                             (kvv[:, h:h + 2, 64:128], (2, 64))], "mla")
            else:
                sw, (wqh, wkh) = wload([(w_qb[l].rearrange("(k p) c -> p k c", p=128)[:, :, 96 * h:96 * h + 96], (2, 96)),
                                        (kvv[:, h, 0:64], (64,))], "mla")
            for g0 in range(0, len(s.tiles), 4):
                grp = s.tiles[g0:g0 + 4]
                ng = len(grp)
                n = grp[0].rows
                b = nba()
                for i, tl in enumerate(grp):
                    mm_group(ps[0:n, b, 96 * i:96 * i + 96], [(CQT[:, c, tl.col0:tl.col0 + n], wqh[:, c, :]) for c in range(2)],
                             [("CQT", tl.col0 // 128), WK(sw), "LNG", "LNB"], [("ps", b)])
                pv = ps[0:n, b, 0:96 * ng].rearrange("p (t c) -> p t c", c=96)
                act_copy(QS[0:n, 0:ng, 0:64], pv[:, :, 0:64], [("ps", b)], [("QS", 0)])
                cs_ = COS[0:n, grp[0].kt:grp[0].kt + ng, :]
                sn_ = SIN[0:n, grp[0].kt:grp[0].kt + ng, :]
                T1 = RT[0:n, 0:16 * ng].rearrange("p (t c) -> p t c", c=16)
                T2 = RT[0:n, 64:64 + 16 * ng].rearrange("p (t c) -> p t c", c=16)
                dve_tt(T1, pv[:, :, 64:80], cs_, ALU.mult, [("ps", b), "!const"], [("RT", 0)])
                dve_tt(T2, pv[:, :, 80:96], sn_, ALU.mult, [("ps", b), "!const"], [("RT", 1)])
                dve_tt(QS[0:n, 0:ng, 64:80], T1, T2, ALU.subtract, [("RT", 0), ("RT", 1)], [("QS", 1)])
                dve_tt(T1, pv[:, :, 80:96], cs_, ALU.mult, [("ps", b), "!const"], [("RT", 0)])
                dve_tt(T2, pv[:, :, 64:80], sn_, ALU.mult, [("ps", b), "!const"], [("RT", 1)])
                dve_tt(QS[0:n, 0:ng, 80:96], T1, T2, ALU.add, [("RT", 0), ("RT", 1)], [("QS", 2)])
                b2 = nba()
                tr_group([(ps[0:96, b2, 128 * i:128 * i + n], QS[0:n, i, :], n) for i in range(ng)], [("QS", 0), ("QS", 1), ("QS", 2)], [("ps", b2)])
                qc0 = grp[0].col0 if s.kind == "p" else 0
                if n == 128:
                    act_copy(QT[0:96, qc0:qc0 + 128 * ng], ps[0:96, b2, 0:128 * ng], [("ps", b2)], ["QT"])
                else:
                    act_copy(QT[0:96, qc0:qc0 + n], ps[0:96, b2, 0:n], [("ps", b2)], ["QT"])
            ktot = npast + s.n_new
            for c0 in range(0, ktot, 512):
                n_ = min(512, ktot - c0)
                b = nba()
                mm_group(ps[0:64, b, 0:n_], [(wkh, CKVT[:, c0:c0 + n_])], ckv_keys + [WK(sw)], [("ps", b)])
                dve_copy(KT[0:64, c0:c0 + n_], ps[0:64, b, 0:n_], [("ps", b)], [("KT", "lo")])
            if hp == 0:
                for g0 in range(0, nkt, 4):
                    grp = kbs[g0:g0 + 4]
                    b = nba()
                    for i, (kpos0, nk, kt) in enumerate(grp):
                        mm_group(ps[0:nk, b, 128 * i:128 * i + 128], [(CKVT[:, kpos0:kpos0 + nk], wvp.rearrange("p a b -> p (a b)"))],
                                 ckv_keys + [WK(sw)], [("ps", b)])
                    for i, (kpos0, nk, kt) in enumerate(grp):
                        if i == 0 or nk != 128:
                            cntf = sum(1 for g_ in grp if g_[1] == 128) if (i == 0 and nk == 128) else 1
                            act_copy(VP[0:nk, kt:kt + cntf, :], ps[0:nk, b, 128 * i:128 * (i + cntf)].rearrange("p (t c) -> p t c", c=128),
                                     [("ps", b)], ["VP"])
            attention(s, hp, (0, 96), QSCALE_M, False, None, OTM, "m", j, [("KT", "lo"), ("KT", "hi")])

    def ffn(R, l, last):
        P.phase = f'ffn-ple {R.kind} l{l}'
        sp_dma(LNG[:], ln_ffn_g[l:l + 1, :].partition_broadcast(128), (), ["LNG"], "io4")
        sp_dma(LNB[:], ln_ffn_b[l:l + 1, :].partition_broadcast(128), (), ["LNB"], "io5")
        for qd in range(4):
            spg, (wpg_,) = wload([(w_pg[l].rearrange("(k p) c -> p k c", p=128)[:, :, 256 * qd:256 * qd + 256], (KC, 256))], "wpg")
            spp, (wpp_,) = wload([(w_pp[l].rearrange("(k p) c -> p k c", p=128)[:, :, 256 * qd:256 * qd + 256], (2, 256))], "wpp")
            for tl in R.tiles:
                n = tl.rows
                cols = slice(tl.col0, tl.col0 + n)
                pi = state["pin"]
                state["pin"] = (pi + 1) % 2
                sp_dma(PIN[pi][0:n, :], psrc(R, l, tl), (), [("STG", pi)], f"st{pi}")
                b = nb()
                tr_group([(ps[:, b, 128 * c:128 * c + n], PIN[pi][0:n, 128 * c:128 * c + 128], n) for c in range(2)], [("STG", pi)], [("ps", b)])
                act_copy(PTT[:, :, 0:n], ps[:, b, 0:256].rearrange("p (c t) -> p c t", c=2)[:, :, 0:n], [("ps", b)], ["PTT"])
                bg = nb()
                mm_group(ps[0:n, bg, 0:256], [(B[:, k, cols], wpg_[:, k, :]) for k in range(KC)], Bkeys(tl) + [WK(spg)], [("ps", bg)])
                mm_group(ps[0:n, bg, 256:512], [(PTT[:, c, 0:n], wpp_[:, c, :]) for c in range(2)], ["PTT", WK(spp)], [("ps", bg)])
                act_fn(SC2[0:n, 0:256], ps[0:n, bg, 0:256], AF.Sigmoid, [("ps", bg)], [("SC2", 0), ("SC2", 1)])
                dve_tt(SC2[0:n, 0:256], SC2[0:n, 0:256], ps[0:n, bg, 256:512], ALU.mult, [("SC2", 0), ("ps", bg)], [("SC2", 0), ("SC2", 1)])
                asl = A[0:n, tl.t, 256 * qd:256 * qd + 256]
                dve_stt(asl, asl, ALPHA, SC2[0:n, 0:256], ALU.mult, ALU.add, [("A", tl.t), ("SC2", 0)], [("A", tl.t)])
        P.phase = f'ffn-swiglu {R.kind} l{l}'
        experts = [None] if l % 2 == 0 else list(range(NE))
        gi = 0
        pending = []

        def flush(k):
            for _ in range(min(k, len(pending))):
                pending.pop(0)()

        for ex in experts:
            if ex is None:
                wg_d = w_fg[0].rearrange("(k p) f -> p k f", p=128)
                wu_d = w_fu[0].rearrange("(k p) f -> p k f", p=128)
                wd_d = w_fd[0].rearrange("(c p) d -> p c d", p=128)
            else:
                wg_d = w_mg[0, ex].rearrange("(k p) f -> p k f", p=128)
                wu_d = w_mu[0, ex].rearrange("(k p) f -> p k f", p=128)
                wd_d = w_md[0, ex].rearrange("(c p) d -> p c d", p=128)
            for f0 in range(0, NFC, 2):
                nf = min(2, NFC - f0)
                sg_, (wg_,) = wload([(wg_d[:, :, 128 * f0:128 * (f0 + nf)], (KC, 128 * nf))], "wg")
                su_, (wu_,) = wload([(wu_d[:, :, 128 * f0:128 * (f0 + nf)], (KC, 128 * nf))], "wu")
                sd_, (wd_,) = wload([(wd_d[:, f0:f0 + nf, :], (nf, D))], "wd")
                AT = ACT_[gi % 2]
                ai = gi % 2
                gi += 1
                g_iters = [(fl, blk) for fl in range(nf) for blk in R.blocks]
                per = (len(pending) + len(g_iters) - 1) // len(g_iters)
                for (fl, (c0, n_, tls)) in g_iters:
                    ws = slice(128 * fl, 128 * fl + 128)
                    cols = slice(c0, c0 + n_)
                    bg, bu = nb(), nb()
                    mm_group(ps[:, bg, 0:n_], [(wg_[:, k, ws], B[:, k, cols]) for k in range(KC)], Bkeys_cols(c0, n_) + [WK(sg_)], [("ps", bg)])
                    mm_group(ps[:, bu, 0:n_], [(wu_[:, k, ws], B[:, k, cols]) for k in range(KC)], Bkeys_cols(c0, n_) + [WK(su_)], [("ps", bu)])
                    act_fn(SC[:, 0:n_], ps[:, bg, 0:n_], AF.Silu, [("ps", bg)], [("SC", 0)])
                    dve_tt(AT[:, fl, cols], SC[:, 0:n_], ps[:, bu, 0:n_], ALU.mult, [("SC", 0), ("ps", bu)], [("ACT", ai, fl, c0)])
                    flush(per)
                flush(len(pending))

                def d_bank(tl, hh, AT=AT, ai=ai, nf=nf, wd_=wd_, sd_=sd_, ex=ex):
                    n = tl.rows
                    bc0 = [c0 for (c0, n_, _) in R.blocks if c0 <= tl.col0 < c0 + n_][0]
                    b = nb()
                    mm_group(ps[0:n, b, :], [(AT[:, fl, tl.col0:tl.col0 + n], wd_[:, fl, 512 * hh:512 * hh + 512]) for fl in range(nf)],
                             [("ACT", ai, fl, bc0) for fl in range(nf)] + [WK(sd_)], [("ps", b)])
                    asl = A[0:n, tl.t, 512 * hh:512 * hh + 512]
                    if ex is None:
                        dve_tt(asl, asl, ps[0:n, b, :], ALU.add, [("A", tl.t), ("ps", b)], [("A", tl.t)])
                    else:
                        dve_stt(asl, ps[0:n, b, :], GATES[0:n, tl.t, ex:ex + 1], asl, ALU.mult, ALU.add,
                                [("A", tl.t), ("ps", b), ("GATES", tl.t)], [("A", tl.t)])
                for tl in R.tiles:
                    for hh in range(2):
                        pending.append(lambda tl=tl, hh=hh, f_=d_bank: f_(tl, hh))
        flush(len(pending))
        P.phase = f'ln2 {R.kind} l{l}'
        for tl in R.tiles:
            n = tl.rows
            layer_norm_tile(tl, from_A=True)
            if last:
                sp_dma(ysrc(R, tl), A[0:n, tl.t, :], [("A", tl.t)], (), f"y{tl.t % 4}")
            else:
                tile_to_B(tl)

    rounds = [make_prompt_round(i) for i in range(NP)] + ([make_sample_round()] if NS else [])
    for R in rounds:
        for tl in R.tiles:
            xl = state["xl"]
            state["xl"] = (xl + 1) % 4
            sp_dma(A[0:tl.rows, tl.t, :], xsrc(R, tl), (), [("A", tl.t)], f"x{xl}")
            tile_to_B(tl)
        for l in range(DEPTH):
            mixer(R, l)
            ffn(R, l, last=(l == DEPTH - 1))

    counts = P.finalize(nc, es)
    es.close()
    return nc, counts


def make_consts(NPT):
    c = np.zeros((128, 512 + 2 * NPT * 16), np.float32)
    c[:, 0:128] = np.eye(128, dtype=np.float32)
    i = np.arange(128)
    c[:, 128:256] = (i[:, None] <= i[None, :]).astype(np.float32)
    c[:, 256:384] = 1.0
    c[:, 384:512] = ((i[:, None] // 64) <= (i[None, :] // 64)).astype(np.float32)
    half = 16
    inv = (np.float32(10000.0) ** (-np.arange(half, dtype=np.float32) * np.float32(2.0) / np.float32(32))).astype(np.float32)
    pos = np.arange(NPT * 128, dtype=np.float32)
    ang = (pos[:, None] * inv[None, :]).astype(np.float32)
    cos = np.cos(ang).astype(np.float32).reshape(NPT, 128, 16).transpose(1, 0, 2).reshape(128, NPT * 16)
    sin = np.sin(ang).astype(np.float32).reshape(NPT, 128, 16).transpose(1, 0, 2).reshape(128, NPT * 16)
    c[:, 512:512 + NPT * 16] = cos
    c[:, 512 + NPT * 16:] = sin
    return c


_CACHE = {}


def run(inputs, n_cores):
    x_prompt = np.asarray(inputs["x_prompt"])
    x_sample = np.asarray(inputs["x_sample"])
    BATCH, SEQ, _ = x_prompt.shape
    DECB, _, _ = x_sample.shape
    PAST = inputs["cache_fox_k"].shape[2]
    DFF = inputs["w_ffn_gate"].shape[2]
    NP = BATCH // n_cores
    NS = DECB // n_cores
    key = (SEQ, NP, NS, PAST, DFF)
    if key not in _CACHE:
        _CACHE[key] = build_program(*key)
    nc, counts = _CACHE[key]
    NKT = max(SEQ // 128, PAST // 128 + 1)
    consts = make_consts(NKT)
    params = np.concatenate([np.asarray(inputs["b_fox_f"], np.float32).ravel(), np.asarray(inputs["g_mla_cq"], np.float32).ravel(),
                             np.asarray(inputs["g_mla_ckv"], np.float32).ravel(), np.asarray(inputs["b_router"], np.float32).ravel()])[None, :]
    shared = {k: np.ascontiguousarray(np.asarray(inputs[k], np.float32)) for k in
              ("w_in", "w_mla_qb", "w_mla_kvb", "w_o_fox", "w_o_mla", "w_out", "ln_mix_g", "ln_mix_b", "w_ffn_gate", "w_ffn_up",
               "w_ffn_down", "w_router", "w_moe_gate", "w_moe_up", "w_moe_down", "w_ple_proj", "w_ple_gate", "ln_ffn_g", "ln_ffn_b")}
    shared["params"] = np.ascontiguousarray(params)
    shared["consts"] = consts
    in_maps = []
    for c in range(n_cores):
        m = dict(shared)
        ps_, ss_ = slice(c * NP, (c + 1) * NP), slice(c * NS, (c + 1) * NS)
        m["x_prompt"] = np.ascontiguousarray(x_prompt[ps_])
        m["x_sample"] = np.ascontiguousarray(x_sample[ss_])
        m["cache_fox_k"] = np.ascontiguousarray(np.asarray(inputs["cache_fox_k"])[:, ss_].reshape(DEPTH, NS, PAST, 512))
        m["cache_fox_v"] = np.ascontiguousarray(np.asarray(inputs["cache_fox_v"])[:, ss_].reshape(DEPTH, NS, PAST, 512))
        m["cache_fox_logf"] = np.ascontiguousarray(np.asarray(inputs["cache_fox_logf"])[:, ss_])
        m["cache_mla_ckv"] = np.ascontiguousarray(np.asarray(inputs["cache_mla_ckv"])[:, ss_])
        m["cache_mla_krope"] = np.ascontiguousarray(np.asarray(inputs["cache_mla_krope"])[:, ss_])
        m["p_prompt"] = np.ascontiguousarray(np.asarray(inputs["p_prompt"])[:, ps_])
        m["p_sample"] = np.ascontiguousarray(np.asarray(inputs["p_sample"])[:, ss_])
        in_maps.append(m)
    res = run_bass_kernel_spmd(nc, in_maps, core_ids=list(range(n_cores)))
    rs = res.results

    def cat(name, axis, shape_tail=None):
        a = np.concatenate([np.asarray(r[name]) for r in rs], axis=axis)
        return a

    y_p = cat("y_prompt", 0)
    y_s = cat("y_sample", 0)
    outs = [y_p, y_s]
    for sfx, nb_, sl in (("prompt", BATCH, SEQ), ("sample", DECB, DS)):
        fk = cat(f"fox_k_{sfx}", 1).reshape(DEPTH, nb_, sl, NH, 64)
        fv = cat(f"fox_v_{sfx}", 1).reshape(DEPTH, nb_, sl, NH, 64)
        lf = cat(f"fox_logf_{sfx}", 1)
        ck = cat(f"mla_ckv_{sfx}", 1)
        kr = cat(f"mla_krope_{sfx}", 1)
        outs += [fk, fv, lf, ck, kr]
    return tuple(np.ascontiguousarray(o.astype(np.float32)) for o in outs)


def kernel(**inputs):
    return run(inputs, 8)
```
